# Optimizing a Trainium2 kernel written in Bass

```python
import math
import jax, jax.numpy as jnp
from jax import lax
import numpy as np

D_MODEL = 1024
BATCH = 1
SEQ = 16384
DEPTH = 4
DEC_BATCH = 2
DEC_SEQ = 8192
PAST_LEN = 128

GRID_W = 64
N_MIXERS = 2
HEAD_DIM = 64
N_HEADS = D_MODEL // HEAD_DIM
N_KV_HEADS = N_HEADS // 4
GQA_GROUP = N_HEADS // N_KV_HEADS
QKV_DIM = (N_HEADS + 2 * N_KV_HEADS) * HEAD_DIM
Q_BLOCK = 128
ROPE_THETA = 10000.0
AXIS_DIM = HEAD_DIM // 2
NA_HEADS = D_MODEL // HEAD_DIM
NA_ROWS_MAX = 8
NA_COLS = 16
N_EXPERTS = 32
TOP_K = 4
D_FF = D_MODEL
SWIGLU_LIMIT = 7.0
SWIGLU_ALPHA = 1.702
MOE_CHUNK = 128
DN_ALPHA = (2 * DEPTH) ** 0.25
DN_BETA = (8 * DEPTH) ** -0.25
N_GQA_LAYERS = (DEPTH + 1) // 2
N_NA_LAYERS = DEPTH // 2
LN_EPS = 1e-5
RMS_EPS = 1e-6

kernel_name = "hybrid_gqa_natten_moe_deepnorm_encoder"


def layer_norm(x, g, b):
    xf = x.astype(jnp.float32)
    mu = jnp.mean(xf, axis=-1, keepdims=True)
    var = jnp.mean(jnp.square(xf - mu), axis=-1, keepdims=True)
    y = (xf - mu) * lax.rsqrt(var + LN_EPS) * g.astype(jnp.float32) + b.astype(jnp.float32)
    return y.astype(x.dtype)


def rms_norm(x, g):
    xf = x.astype(jnp.float32)
    y = xf * lax.rsqrt(jnp.mean(jnp.square(xf), axis=-1, keepdims=True) + RMS_EPS) * g.astype(jnp.float32)
    return y.astype(x.dtype)


def axial_angles(seq_len):
    t = jnp.arange(seq_len)
    row = (t // GRID_W).astype(jnp.float32)
    col = (t % GRID_W).astype(jnp.float32)
    freqs = ROPE_THETA ** (-jnp.arange(0, AXIS_DIM, 2, dtype=jnp.float32) / AXIS_DIM)
    return row[:, None] * freqs, col[:, None] * freqs


def rope_half(x, ang):
    c = jnp.cos(ang)[None, :, None, :].astype(x.dtype)
    s = jnp.sin(ang)[None, :, None, :].astype(x.dtype)
    half = AXIS_DIM // 2
    x1, x2 = x[..., :half], x[..., half:]
    return jnp.concatenate([x1 * c - x2 * s, x2 * c + x1 * s], axis=-1)


def axial_rope(x, ang_row, ang_col):
    return jnp.concatenate([rope_half(x[..., :AXIS_DIM], ang_row),
                            rope_half(x[..., AXIS_DIM:], ang_col)], axis=-1)


def gqa_mixer(x, w_qkv, q_g, k_g, w_o):
    B, S, _ = x.shape
    qkv = x @ w_qkv
    nq_cols = N_HEADS * HEAD_DIM
    nk_cols = N_KV_HEADS * HEAD_DIM
    q = qkv[..., :nq_cols].reshape(B, S, N_HEADS, HEAD_DIM)
    k = qkv[..., nq_cols:nq_cols + nk_cols].reshape(B, S, N_KV_HEADS, HEAD_DIM)
    v = qkv[..., nq_cols + nk_cols:].reshape(B, S, N_KV_HEADS, HEAD_DIM)
    q = rms_norm(q, q_g)
    k = rms_norm(k, k_g)
    ang_r, ang_c = axial_angles(S)
    q = axial_rope(q, ang_r, ang_c) * (HEAD_DIM ** -0.5)
    k = axial_rope(k, ang_r, ang_c)
    n_blk = S // Q_BLOCK
    qb = q.reshape(B, n_blk, Q_BLOCK, N_KV_HEADS, GQA_GROUP, HEAD_DIM).transpose(1, 0, 2, 3, 4, 5)

    def block(q_blk):
        s = jnp.einsum('bqkgd,bskd->bkgqs', q_blk, k).astype(jnp.float32)
        p = jax.nn.softmax(s, axis=-1).astype(v.dtype)
        return jnp.einsum('bkgqs,bskd->bqkgd', p, v)

    o = lax.map(block, qb)
    o = o.transpose(1, 0, 2, 3, 4, 5).reshape(B, S, N_HEADS * HEAD_DIM)
    return o @ w_o


def neighbourhood_mixer(x, w_qkv, rpb, w_o):
    B, S, _ = x.shape
    rows = S // GRID_W
    kr = min(NA_ROWS_MAX, rows)
    qkv = (x @ w_qkv).reshape(B, rows, GRID_W, 3, NA_HEADS, HEAD_DIM)
    q = qkv[..., 0, :, :] * (HEAD_DIM ** -0.5)
    k = qkv[..., 1, :, :]
    v = qkv[..., 2, :, :]
    cols = jnp.arange(GRID_W)
    col_start = jnp.clip(cols - NA_COLS // 2, 0, GRID_W - NA_COLS)
    col_idx = col_start[:, None] + jnp.arange(NA_COLS)
    dc_idx = col_idx - cols[:, None] + (NA_COLS - 1)
    bias_c = rpb[:, :, dc_idx]

    def row_block(r):
        rs = jnp.clip(r - kr // 2, 0, rows - kr)
        q_r = lax.dynamic_index_in_dim(q, r, axis=1, keepdims=False)
        k_n = lax.dynamic_slice_in_dim(k, rs, kr, axis=1)[:, :, col_idx]
        v_n = lax.dynamic_slice_in_dim(v, rs, kr, axis=1)[:, :, col_idx]
        dr_idx = rs + jnp.arange(kr) - r + (NA_ROWS_MAX - 1)
        bias = jnp.take(bias_c, dr_idx, axis=1).transpose(0, 2, 1, 3)
        s = jnp.einsum('bwhd,biwjhd->bhwij', q_r, k_n).astype(jnp.float32) + bias[None].astype(jnp.float32)
        p = jax.nn.softmax(s.reshape(B, NA_HEADS, GRID_W, kr * NA_COLS), axis=-1)
        p = p.reshape(B, NA_HEADS, GRID_W, kr, NA_COLS).astype(v.dtype)
        return jnp.einsum('bhwij,biwjhd->bwhd', p, v_n)

    o = lax.map(row_block, jnp.arange(rows))
    o = jnp.moveaxis(o, 0, 1).reshape(B, S, NA_HEADS * HEAD_DIM)
    return o @ w_o


def moe(x, w_r, b_r, w_gu, b_gu, w_dn, b_dn):
    B, S, D = x.shape
    xt = x.reshape(-1, D)
    T = xt.shape[0]
    logits = (xt @ w_r).astype(jnp.float32) + b_r.astype(jnp.float32)
    vals, idx = lax.top_k(logits, TOP_K)
    wts = jax.nn.softmax(vals, axis=-1)
    gates = jnp.sum(jax.nn.one_hot(idx, N_EXPERTS, dtype=jnp.float32) * wts[..., None], axis=1).astype(x.dtype)
    n_chunk = T // MOE_CHUNK

    def chunk(args):
        xc, gc = args
        h = jnp.einsum('td,edf->tef', xc, w_gu) + b_gu
        g = jnp.minimum(h[..., 0::2], SWIGLU_LIMIT)
        u = jnp.clip(h[..., 1::2], -SWIGLU_LIMIT, SWIGLU_LIMIT)
        a = (u + 1.0) * (g * jax.nn.sigmoid(SWIGLU_ALPHA * g)) * gc[..., None]
        return jnp.einsum('tef,efd->td', a, w_dn) + gc @ b_dn

    out = lax.map(chunk, (xt.reshape(n_chunk, MOE_CHUNK, D), gates.reshape(n_chunk, MOE_CHUNK, N_EXPERTS)))
    return out.reshape(B, S, D)


def trunk(x, gqa_w_qkv, gqa_q_norm, gqa_k_norm, gqa_w_o, na_w_qkv, na_rpb, na_w_o,
          ln1_g, ln1_b, ln2_g, ln2_b, router_w, router_b, exp_w_gu, exp_b_gu, exp_w_down, exp_b_down):
    for i in range(DEPTH):
        j = i // N_MIXERS
        if i % N_MIXERS == 0:
            h = gqa_mixer(x, gqa_w_qkv[j], gqa_q_norm[j], gqa_k_norm[j], gqa_w_o[j])
        else:
            h = neighbourhood_mixer(x, na_w_qkv[j], na_rpb[j], na_w_o[j])
        x = layer_norm(DN_ALPHA * x + h, ln1_g[i], ln1_b[i])
        f = moe(x, router_w[i], router_b[i], exp_w_gu[i], exp_b_gu[i], exp_w_down[i], exp_b_down[i])
        x = layer_norm(DN_ALPHA * x + f, ln2_g[i], ln2_b[i])
    return x


def setup_inputs(seed: int = 0) -> dict:
    key = jax.random.key(seed)
    ks = jax.random.split(key, 20)
    f32 = jnp.float32
    s_in = D_MODEL ** -0.5
    gqa_col_scale = jnp.concatenate([jnp.ones(((N_HEADS + N_KV_HEADS) * HEAD_DIM,), f32),
                                     jnp.full((N_KV_HEADS * HEAD_DIM,), DN_BETA, f32)])
    na_col_scale = jnp.concatenate([jnp.ones((2 * D_MODEL,), f32), jnp.full((D_MODEL,), DN_BETA, f32)])
    return {
        "x_prompt": jax.random.normal(ks[0], (BATCH, SEQ, D_MODEL), f32),
        "x_sample": jax.random.normal(ks[1], (DEC_BATCH, DEC_SEQ, D_MODEL), f32),
        "gqa_w_qkv": jax.random.normal(ks[2], (N_GQA_LAYERS, D_MODEL, QKV_DIM), f32) * s_in * gqa_col_scale,
        "gqa_q_norm": 1.0 + 0.02 * jax.random.normal(ks[3], (N_GQA_LAYERS, HEAD_DIM), f32),
        "gqa_k_norm": 1.0 + 0.02 * jax.random.normal(ks[4], (N_GQA_LAYERS, HEAD_DIM), f32),
        "gqa_w_o": jax.random.normal(ks[5], (N_GQA_LAYERS, D_MODEL, D_MODEL), f32) * s_in * DN_BETA,
        "na_w_qkv": jax.random.normal(ks[6], (N_NA_LAYERS, D_MODEL, 3 * D_MODEL), f32) * s_in * na_col_scale,
        "na_rpb": 0.02 * jax.random.normal(ks[7], (N_NA_LAYERS, NA_HEADS, 2 * NA_ROWS_MAX - 1, 2 * NA_COLS - 1), f32),
        "na_w_o": jax.random.normal(ks[8], (N_NA_LAYERS, D_MODEL, D_MODEL), f32) * s_in * DN_BETA,
        "ln1_g": 1.0 + 0.02 * jax.random.normal(ks[9], (DEPTH, D_MODEL), f32),
        "ln1_b": 0.02 * jax.random.normal(ks[10], (DEPTH, D_MODEL), f32),
        "ln2_g": 1.0 + 0.02 * jax.random.normal(ks[11], (DEPTH, D_MODEL), f32),
        "ln2_b": 0.02 * jax.random.normal(ks[12], (DEPTH, D_MODEL), f32),
        "router_w": jax.random.normal(ks[13], (DEPTH, D_MODEL, N_EXPERTS), f32) * s_in,
        "router_b": 0.01 * jax.random.normal(ks[14], (DEPTH, N_EXPERTS), f32),
        "exp_w_gu": jax.random.normal(ks[15], (DEPTH, N_EXPERTS, D_MODEL, 2 * D_FF), f32) * s_in,
        "exp_b_gu": 0.01 * jax.random.normal(ks[16], (DEPTH, N_EXPERTS, 2 * D_FF), f32),
        "exp_w_down": jax.random.normal(ks[17], (DEPTH, N_EXPERTS, D_FF, D_MODEL), f32) * (D_FF ** -0.5) * DN_BETA,
        "exp_b_down": 0.01 * jax.random.normal(ks[18], (DEPTH, N_EXPERTS, D_MODEL), f32),
    }


def reference(x_prompt, x_sample, gqa_w_qkv, gqa_q_norm, gqa_k_norm, gqa_w_o, na_w_qkv, na_rpb, na_w_o,
              ln1_g, ln1_b, ln2_g, ln2_b, router_w, router_b, exp_w_gu, exp_b_gu, exp_w_down, exp_b_down):
    y_prompt = trunk(x_prompt, gqa_w_qkv, gqa_q_norm, gqa_k_norm, gqa_w_o, na_w_qkv, na_rpb, na_w_o,
                     ln1_g, ln1_b, ln2_g, ln2_b, router_w, router_b, exp_w_gu, exp_b_gu, exp_w_down, exp_b_down)
    y_sample = trunk(x_sample, gqa_w_qkv, gqa_q_norm, gqa_k_norm, gqa_w_o, na_w_qkv, na_rpb, na_w_o,
                     ln1_g, ln1_b, ln2_g, ln2_b, router_w, router_b, exp_w_gu, exp_b_gu, exp_w_down, exp_b_down)
    return (y_prompt, y_sample)
```

```python
import contextlib
import os
PH = os.environ.get('KPH', '12345')
import numpy as np
import ml_dtypes
import concourse.bass as bass
import concourse.mybir as mybir
from concourse.bass_utils import run_bass_kernel_spmd

F32 = mybir.dt.float32
BF16 = mybir.dt.bfloat16
AF = mybir.ActivationFunctionType
ALU = mybir.AluOpType
AX = mybir.AxisListType

D = 1024
GW = 64
HD = 64
NH = 16
NKV = 4
LN_EPS = 1e-5
RMS_EPS = 1e-6
NEG = -30000.0


class Res:
    __slots__ = ("w", "r", "name")

    def __init__(self, name=""):
        self.w = {}
        self.r = {}
        self.name = name


class DSem:
    def __init__(self, sem, name):
        self.sem = sem
        self.total = 0
        self.name = name


class KB:
    def __init__(self, nc):
        self.nc = nc
        self.eng = {"pe": nc.tensor, "act": nc.scalar, "dve": nc.vector, "pool": nc.gpsimd, "sp": nc.sync}
        self.esem = {}
        for k in ("pe", "act", "dve", "pool"):
            self.esem[k] = nc.alloc_semaphore(name=f"prog_{k}")
        self.cnt = {k: 0 for k in self.esem}
        self.known = {k: {} for k in self.eng}
        self.dsems = []
        self.free_ds = []
        self.live_ds = []
        self.uid = 0

    def dsem(self, name):
        if self.free_ds:
            d = self.free_ds.pop()
        else:
            d = DSem(self.nc.alloc_semaphore(name=f"d_{name}_{len(self.dsems)}"), name)
            self.dsems.append(d)
        self.live_ds.append(d)
        return d

    def release_ds(self, keep=()):
        for d in self.live_ds:
            if d not in keep:
                self.free_ds.append(d)
        self.live_ds = [d for d in self.live_ds if d in keep]

    def _wait(self, e, key, sem, val):
        if val <= 0:
            return
        if key == e:
            return
        if self.known[e].get(key, 0) >= val:
            return
        self.eng[e].wait_ge(sem, val)
        self.known[e][key] = val

    def _deps(self, e, reads, writes, disjoint):
        for R in reads:
            for key, (sem, val) in R.w.items():
                if key == e and e != "pe":
                    if self.known[e].get("self", 0) < val:
                        self.eng[e].wait_ge(sem, val)
                        self.known[e]["self"] = val
                    continue
                self._wait(e, key, sem, val)
        for R in writes:
            for key, (sem, val) in R.r.items():
                self._wait(e, key, sem, val)
            if not disjoint:
                for key, (sem, val) in R.w.items():
                    self._wait(e, key, sem, val)

    def _record(self, key, sem, val, reads, writes, disjoint):
        for R in reads:
            R.r[key] = (sem, val)
        for R in writes:
            if R.r or not disjoint:
                R.w = {}
                R.r = {}
            R.w[key] = (sem, val)

    def op(self, e, fn, reads=(), writes=(), disjoint=False):
        self._deps(e, reads, writes, disjoint)
        ins = fn(self.eng[e])
        self.cnt[e] += 1
        ins.then_inc(self.esem[e], 1)
        self._record(e, self.esem[e], self.cnt[e], reads, writes, disjoint)
        return ins

    def dma(self, ds, out, in_, reads=(), writes=(), q="sp", disjoint=True):
        self._deps(q, reads, writes, disjoint)
        self._wait(q, id(ds), ds.sem, ds.total)
        ins = self.eng[q].dma_start(out=out, in_=in_)
        ds.total += 16
        ins.then_inc(ds.sem, 16)
        self._record(id(ds), ds.sem, ds.total, reads, writes, disjoint)

    def barrier(self):
        evs = [(k, self.esem[k], self.cnt[k]) for k in self.esem]
        evs += [(id(d), d.sem, d.total) for d in self.dsems]
        for e in self.eng:
            for key, sem, val in evs:
                self._wait(e, key, sem, val)

    def final_wait(self):
        for d in self.dsems:
            self._wait("sp", id(d), d.sem, d.total)


def build_program(T, E, DEPTH, alpha, li0=0):
    NT = T // 128
    NC = T // 128
    NB = T // 256
    n_gqa = sum(1 for l in range(li0, li0 + DEPTH) if l % 2 == 0)
    n_na = DEPTH - n_gqa
    BLK = 512
    nc = bass.Bass("TRN2", target_bir_lowering=False)
    kb = KB(nc)

    def din(name, shape, dt=F32):
        return nc.dram_tensor(name, list(shape), dt, kind="ExternalInput").ap()

    def dscr(name, shape, dt):
        return nc.dram_tensor(name, list(shape), dt, kind="Internal").ap()

    x_in = din("x", [T, D])
    gqa_w_qkv = din("gqa_w_qkv", [max(n_gqa, 1), D, 1536])
    gqa_qn = din("gqa_q_norm", [max(n_gqa, 1), HD])
    gqa_kn = din("gqa_k_norm", [max(n_gqa, 1), HD])
    gqa_w_o = din("gqa_w_o", [max(n_gqa, 1), D, D])
    na_w_qkv = din("na_w_qkv", [max(n_na, 1), D, 3 * D])
    na_m = din("na_m", [max(n_na, 1), NH, 15, 64, 64])
    na_w_o = din("na_w_o", [max(n_na, 1), D, D])
    ln1_g = din("ln1_g", [DEPTH, D]); ln1_b = din("ln1_b", [DEPTH, D])
    ln2_g = din("ln2_g", [DEPTH, D]); ln2_b = din("ln2_b", [DEPTH, D])
    router_w = din("router_w", [DEPTH, D, E]); router_b = din("router_b", [DEPTH, E])
    w_gu = din("exp_w_gu", [DEPTH, E, D, 2 * D]); b_gu = din("exp_b_gu", [DEPTH, E, 2 * D])
    w_dn = din("exp_w_down", [DEPTH, E, D, D]); b_dn = din("exp_b_down", [DEPTH, E, D])
    rope_c = din("rope_c", [T, 64]); rope_s = din("rope_s", [T, 64])
    tv_in = din("tv", [128, NC])
    na_rv = din("na_rv", [4, 128, 6 * 256], BF16)
    na_mc = din("na_mc", [2, 128, 64])
    ident_f_in = din("ident_f", [128, 128])
    y_out = nc.dram_tensor("y", [T, D], F32, kind="ExternalOutput").ap()

    xres = dscr("xres", [T, D], F32)
    x1_d = dscr("x1_d", [T, D], F32)
    QT_d = dscr("QT_d", [HD, NH, T], BF16)
    KT_d = dscr("KT_d", [HD, NH, T], BF16)
    V_d = dscr("V_d", [T, NH * HD], BF16)
    OT_d = dscr("OT_d", [128, 8, T], BF16)
    x1T_d = dscr("x1T_d", [128, 8, T], BF16)
    gates_d = dscr("gates_d", [T, E], F32)
    acci_d = dscr("acci_d", [T, D], F32)
    wgu_b = dscr("wgu_b", [E, 128, 8 * 2 * D], BF16)
    wd_b = dscr("wd_b", [E, 128, 8 * D], BF16)

    R_xres = Res("xres"); R_x1 = Res("x1"); R_QT = Res("QT"); R_KT = Res("KT"); R_V = Res("V")
    R_OT = Res("OT"); R_x1T = Res("x1T"); R_gates = Res("gates"); R_acci = Res("acci")
    R_wgu = Res("wgu"); R_wd = Res("wd"); R_y = Res("y")

    names = [0]

    def uname(p):
        names[0] += 1
        return f"{p}_{names[0]}"

    class Phase:
        def __init__(self):
            self.st = contextlib.ExitStack()

        def sb(self, shape, dt, name="t"):
            return self.st.enter_context(nc.sbuf_tensor(uname(name), list(shape), dt))

        def ps(self, shape, dt, name="p"):
            return self.st.enter_context(nc.psum_tensor(uname(name), list(shape), dt))

        def close(self):
            kb.barrier()
            kb.release_ds(keep=(ds_c,))
            self.st.close()

    pc = Phase()
    ident_f = pc.sb([128, 128], F32, "identf")
    ident_b = pc.sb([128, 128], BF16, "identb")
    R_const = Res("const")
    ds_c = kb.dsem("const")
    kb.dma(ds_c, ident_f[:], ident_f_in[:, :], writes=[R_const])
    kb.op("dve", lambda v: v.tensor_copy(out=ident_b[:], in_=ident_f[:]), reads=[R_const], writes=[R_const])

    def load_w_bf16(ph, w_ap, N, dst, R_dst, stage, R_stage, ds_stage):
        for kc in range(8):
            s = kc % 2
            kb.dma(ds_stage[s], stage[s][:, 0:N], w_ap[kc * 128:(kc + 1) * 128, :], writes=[R_stage[s]], disjoint=False)
            eng = "dve" if kc % 2 == 0 else "pool"
            kb.op(eng, lambda v, s=s, kc=kc: v.tensor_copy(out=dst[:, kc, :], in_=stage[s][:, 0:N]),
                  reads=[R_stage[s]], writes=[R_dst], disjoint=True)

    def bcast_load(ds, dst_ap, src_row_ap, n, R_dst):
        kb.dma(ds, dst_ap, src_row_ap.broadcast_to([128, n]), writes=[R_dst])

    def layer_norm(z, R_z, out, R_out, gt, bt, R_tab, tmp):
        st6, mv, rstd, R_t = tmp
        kb.op("dve", lambda v: v.bn_stats(out=st6[:, 0, :], in_=z[:, 0:512]), reads=[R_z], writes=[R_t])
        kb.op("dve", lambda v: v.bn_stats(out=st6[:, 1, :], in_=z[:, 512:1024]), reads=[R_z], writes=[R_t], disjoint=True)
        kb.op("dve", lambda v: v.bn_aggr(out=mv[:], in_=st6[:].rearrange("p a b -> p (a b)")), reads=[R_t], writes=[R_t])
        kb.op("dve", lambda v: v.tensor_scalar(out=rstd[:], in0=mv[:, 1:2], scalar1=LN_EPS, scalar2=None, op0=ALU.add),
              reads=[R_t], writes=[R_t])
        kb.op("act", lambda a: a.activation(out=rstd[:], in_=rstd[:], func=AF.Sqrt), reads=[R_t], writes=[R_t])
        kb.op("dve", lambda v: v.reciprocal(out=rstd[:], in_=rstd[:]), reads=[R_t], writes=[R_t])
        kb.op("dve", lambda v: v.tensor_scalar(out=z[:], in0=z[:], scalar1=mv[:, 0:1], scalar2=rstd[:, 0:1],
                                               op0=ALU.subtract, op1=ALU.mult), reads=[R_z, R_t], writes=[R_z])
        kb.op("pool", lambda v: v.tensor_tensor(out=z[:], in0=z[:], in1=gt[:], op=ALU.mult), reads=[R_z, R_tab], writes=[R_z])
        kb.op("pool", lambda v: v.tensor_tensor(out=out[:], in0=z[:], in1=bt[:], op=ALU.add), reads=[R_z, R_tab], writes=[R_out])

    for li in range(DEPTH):
        is_gqa = ((li0 + li) % 2 == 0)
        lj = sum(1 for l in range(li0, li0 + li) if (l % 2 == 0) == is_gqa)
        src_x = x_in if li == 0 else xres
        R_src = R_const if li == 0 else R_xres
        for sub1 in range(0, NT, 32):
            ph = Phase()
            NQKV = 1536 if is_gqa else 3072
            wq = ph.sb([128, 8, NQKV], BF16, "wqkv")
            R_wq = Res()
            stage = [ph.sb([128, 3072], F32, "stg") for _ in range(2)]
            R_stage = [Res(), Res()]
            ds_stage = [kb.dsem("stg"), kb.dsem("stg")]
            load_w_bf16(ph, (gqa_w_qkv if is_gqa else na_w_qkv)[lj], NQKV, wq, R_wq, stage, R_stage, ds_stage)
            xt = [ph.sb([128, D], F32, "xt") for _ in range(2)]
            R_xt = [Res(), Res()]
            ds_xt = [kb.dsem("xt"), kb.dsem("xt")]
            xb = ph.sb([128, D], BF16, "xb"); R_xb = Res()
            xT = ph.sb([128, 8, 128], BF16, "xT"); R_xT = Res()
            p_xT = ph.ps([128, 8, 128], BF16, "pxT"); R_pxT = Res()
            p_pr = ph.ps([128, 1024], F32, "ppr"); R_ppr = Res()
            p_tr = ph.ps([64, 16, 128], BF16, "ptr"); R_ptr = Res()
            qb = ph.sb([128, 16, 64], BF16, "qb"); R_qb = Res()
            qT = ph.sb([64, 16, 128], BF16, "qT"); R_qT = Res()
            ds_qT = kb.dsem("qT")
            vb = ph.sb([128, 1024], BF16, "vb"); R_vb = Res(); ds_vb = kb.dsem("vb")
            if is_gqa:
                gain = ph.sb([128, 20, 64], F32, "gain"); R_gain = Res(); ds_g = kb.dsem("gain")
                for h in range(20):
                    src = gqa_qn if h < 16 else gqa_kn
                    bcast_load(ds_g, gain[:, h, :], src[lj:lj + 1, :], 64, R_gain)
                ct = [ph.sb([128, 64], F32, "ct") for _ in range(2)]
                sn = [ph.sb([128, 64], F32, "sn") for _ in range(2)]
                R_cs = [Res(), Res()]; ds_cs = [kb.dsem("cs"), kb.dsem("cs")]
                sq = ph.sb([128, 16, 64], F32, "sq"); R_sq = Res()
                ssum = ph.sb([128, 16], F32, "ssum"); R_ss = Res()
                qn = ph.sb([128, 16, 64], F32, "qn"); R_qn = Res()
                t1 = ph.sb([128, 16, 64], F32, "t1"); R_t1 = Res()
                t2 = ph.sb([128, 16, 64], F32, "t2"); R_t2 = Res()

            def project(c0, ncols):
                for n0 in range(0, ncols, 512):
                    nn = min(512, ncols - n0)
                    for kc in range(8):
                        kb.op("pe", lambda pe, n0=n0, nn=nn, kc=kc: pe.matmul(
                            p_pr[:, n0:n0 + nn], lhsT=xT[:, kc, :], rhs=wq[:, kc, c0 + n0:c0 + n0 + nn],
                            start=(kc == 0), stop=(kc == 7)),
                            reads=[R_xT, R_wq], writes=[R_ppr], disjoint=True)

            def transpose_out(nheads, dst_d, R_dst, t0):
                for h in range(nheads):
                    kb.op("pe", lambda pe, h=h: pe.transpose(out=p_tr[:, h, :], in_=qb[:, h, :], identity=ident_b[:]),
                          reads=[R_qb, R_const], writes=[R_ptr], disjoint=True)
                kb.op("act", lambda a: a.activation(func=AF.Copy, out=qT[:, 0:nheads, :], in_=p_tr[:, 0:nheads, :]), reads=[R_ptr], writes=[R_qT])
                kb.dma(ds_qT, dst_d[:, 0:nheads, t0:t0 + 128], qT[:, 0:nheads, :],
                       reads=[R_qT], writes=[R_dst])

            def norm_rope(nheads, g0, s):
                pv = p_pr[:, 0:nheads * 64].rearrange("p (h d) -> p h d", d=64)
                kb.op("act", lambda a: a.activation(out=sq[:, 0:nheads, :], in_=pv, func=AF.Square), reads=[R_ppr], writes=[R_sq])
                kb.op("dve", lambda v: v.tensor_reduce(out=ssum[:, 0:nheads], in_=sq[:, 0:nheads, :], axis=AX.X, op=ALU.add),
                      reads=[R_sq], writes=[R_ss])
                kb.op("dve", lambda v: v.tensor_scalar(out=ssum[:, 0:nheads], in0=ssum[:, 0:nheads], scalar1=1.0 / 64, scalar2=RMS_EPS,
                                                       op0=ALU.mult, op1=ALU.add), reads=[R_ss], writes=[R_ss])
                kb.op("act", lambda a: a.activation(out=ssum[:, 0:nheads], in_=ssum[:, 0:nheads], func=AF.Sqrt), reads=[R_ss], writes=[R_ss])
                kb.op("dve", lambda v: v.reciprocal(out=ssum[:, 0:nheads], in_=ssum[:, 0:nheads]), reads=[R_ss], writes=[R_ss])
                kb.op("dve", lambda v: v.tensor_tensor(out=qn[:, 0:nheads, :], in0=pv,
                                                       in1=ssum[:, 0:nheads].unsqueeze(2).broadcast_to([128, nheads, 64]), op=ALU.mult),
                      reads=[R_ppr, R_ss], writes=[R_qn])
                kb.op("pool", lambda v: v.tensor_tensor(out=qn[:, 0:nheads, :], in0=qn[:, 0:nheads, :], in1=gain[:, g0:g0 + nheads, :], op=ALU.mult),
                      reads=[R_qn, R_gain], writes=[R_qn])
                kb.op("pool", lambda v: v.tensor_tensor(out=t1[:, 0:nheads, :], in0=qn[:, 0:nheads, :],
                                                        in1=ct[s][:].unsqueeze(1).broadcast_to([128, nheads, 64]), op=ALU.mult),
                      reads=[R_qn, R_cs[s]], writes=[R_t1])
                q5 = qn[:, 0:nheads, :].rearrange("p h (a f j) -> p h a f j", a=2, f=2)
                t5 = t2[:, 0:nheads, :].rearrange("p h (a f j) -> p h a f j", a=2, f=2)
                s5 = sn[s][:].rearrange("p (a f j) -> p a f j", a=2, f=2)
                for hf in range(2):
                    kb.op("dve", lambda v, hf=hf: v.tensor_tensor(
                        out=t5[:, :, :, hf, :], in0=q5[:, :, :, 1 - hf, :],
                        in1=s5[:, :, hf, :].unsqueeze(1).broadcast_to([128, nheads, 2, 16]), op=ALU.mult),
                        reads=[R_qn, R_cs[s]], writes=[R_t2], disjoint=True)
                kb.op("dve", lambda v: v.tensor_tensor(out=qb[:, 0:nheads, :], in0=t1[:, 0:nheads, :], in1=t2[:, 0:nheads, :], op=ALU.add),
                      reads=[R_t1, R_t2], writes=[R_qb])

            for t in range(sub1, min(NT, sub1 + 32) if '1' in PH else 0):
                s = t % 2
                t0 = t * 128
                kb.dma(ds_xt[s], xt[s][:], src_x[t0:t0 + 128, :], reads=[R_src], writes=[R_xt[s]], disjoint=False)
                if is_gqa:
                    kb.dma(ds_cs[s], ct[s][:], rope_c[t0:t0 + 128, :], writes=[R_cs[s]], disjoint=False)
                    kb.dma(ds_cs[s], sn[s][:], rope_s[t0:t0 + 128, :], writes=[R_cs[s]], disjoint=True)
                kb.op("pool", lambda v: v.tensor_copy(out=xb[:], in_=xt[s][:]), reads=[R_xt[s]], writes=[R_xb])
                for kc in range(8):
                    kb.op("pe", lambda pe, kc=kc: pe.transpose(out=p_xT[:, kc, :], in_=xb[:, kc * 128:(kc + 1) * 128], identity=ident_b[:]),
                          reads=[R_xb, R_const], writes=[R_pxT], disjoint=True)
                kb.op("act", lambda a: a.activation(func=AF.Copy, out=xT[:], in_=p_xT[:]), reads=[R_pxT], writes=[R_xT])
                if is_gqa:
                    project(0, 1024)
                    norm_rope(16, 0, s)
                    transpose_out(16, QT_d, R_QT, t0)
                    project(1024, 512)
                    norm_rope(4, 16, s)
                    kb.op("act", lambda a: a.activation(func=AF.Copy, out=vb[:, 0:256], in_=p_pr[:, 256:512]), reads=[R_ppr], writes=[R_vb])
                    transpose_out(4, KT_d, R_KT, t0)
                    kb.dma(ds_vb, V_d[t0:t0 + 128, 0:256], vb[:, 0:256], reads=[R_vb], writes=[R_V])
                else:
                    project(0, 1024)
                    kb.op("act", lambda a: a.activation(func=AF.Copy, out=qb[:].rearrange("p h d -> p (h d)"), in_=p_pr[:, :]), reads=[R_ppr], writes=[R_qb])
                    transpose_out(16, QT_d, R_QT, t0)
                    project(1024, 1024)
                    kb.op("act", lambda a: a.activation(func=AF.Copy, out=qb[:].rearrange("p h d -> p (h d)"), in_=p_pr[:, :]), reads=[R_ppr], writes=[R_qb])
                    transpose_out(16, KT_d, R_KT, t0)
                    project(2048, 1024)
                    kb.op("act", lambda a: a.activation(func=AF.Copy, out=vb[:], in_=p_pr[:, :]), reads=[R_ppr], writes=[R_vb])
                    kb.dma(ds_vb, V_d[t0:t0 + 128, :], vb[:], reads=[R_vb], writes=[R_V])
            ph.close()

        hstep = 1 if is_gqa else 4
        for hg in range(0, NH, hstep):
            ph = Phase()
            if is_gqa:
                KT = ph.sb([64, T], BF16, "KT"); R_KTs = Res(); ds_KT = kb.dsem("KT")
                VA = ph.sb([128, NC, 128], BF16, "VA"); R_VA = Res(); ds_VA = kb.dsem("VA")
                tvt = ph.sb([128, NC], F32, "tvt"); R_tv = Res(); ds_tv = kb.dsem("tv")
                QT = [ph.sb([64, T], BF16, "QT") for _ in range(2)]; R_QTs = [Res(), Res()]; ds_QT = [kb.dsem("QT"), kb.dsem("QT")]
                p_s = [ph.ps([128, 2, 512], F32, "ps") for _ in range(2)]; R_ps = [Res(), Res()]
                p_o = [ph.ps([128, 512], F32, "po") for _ in range(2)]; R_po = [Res(), Res()]
                PT = [ph.sb([128, 2, 512], BF16, "PT") for _ in range(3)]; R_PT = [Res(), Res(), Res()]
                rec = ph.sb([128, 512], F32, "rec"); R_rec = Res()
                on = [ph.sb([64, 512], BF16, "on") for _ in range(2)]; R_on = [Res(), Res()]; ds_on = [kb.dsem("on"), kb.dsem("on")]
                kb.dma(ds_tv, tvt[:], tv_in[:, :], writes=[R_tv])
                for c0 in range(0, NC, 32):
                    c1 = min(NC, c0 + 32)
                    kb.op("pool", lambda v, c0=c0, c1=c1: v.memset(VA[:, c0:c1, :], 1.0), writes=[R_VA], disjoint=(c0 > 0))
                gi = 0
                qi = 0
                for h in range(hg, hg + hstep if '2' in PH else hg):
                    g = h // 4
                    if h % 4 == 0 or h == hg:
                        for c0 in range(0, T, 4096):
                            c1 = min(T, c0 + 4096)
                            kb.dma(ds_KT, KT[:, c0:c1], KT_d[:, g, c0:c1], reads=[R_KT], writes=[R_KTs], disjoint=(c0 > 0))
                        for c0 in range(0, NC, 16):
                            c1 = min(NC, c0 + 16)
                            kb.dma(ds_VA, VA[:, c0:c1, 0:64], V_d[c0 * 128:c1 * 128, g * 64:(g + 1) * 64].rearrange("(c p) d -> p c d", p=128),
                                   reads=[R_V], writes=[R_VA], disjoint=(c0 > 0))
                        for c0 in range(0, NC, 32):
                            c1 = min(NC, c0 + 32)
                            kb.op("dve", lambda v, c0=c0, c1=c1: v.tensor_tensor(out=VA[:, c0:c1, :], in0=VA[:, c0:c1, :],
                                                                                 in1=tvt[:, c0:c1].unsqueeze(2).broadcast_to([128, c1 - c0, 128]), op=ALU.mult),
                                  reads=[R_VA, R_tv], writes=[R_VA])
                    hs = h % 2
                    for c0 in range(0, T, 4096):
                        c1 = min(T, c0 + 4096)
                        kb.dma(ds_QT[hs], QT[hs][:, c0:c1], QT_d[:, h, c0:c1], reads=[R_QT], writes=[R_QTs[hs]], disjoint=(c0 > 0))
                    for qt in range(T // 512):
                        po = qi % 2
                        qi += 1
                        for grp in range(NC // 2):
                            sp_ = gi % 2
                            pt_ = gi % 3
                            gi += 1
                            for j in range(2):
                                c = grp * 2 + j
                                kb.op("pe", lambda pe, c=c, j=j: pe.matmul(p_s[sp_][:, j, :], lhsT=KT[:, c * 128:(c + 1) * 128],
                                                                          rhs=QT[hs][:, qt * 512:(qt + 1) * 512], start=True, stop=True),
                                      reads=[R_KTs, R_QTs[hs]], writes=[R_ps[sp_]], disjoint=True)
                            kb.op("act", lambda a: a.activation(out=PT[pt_][:], in_=p_s[sp_][:], func=AF.Exp, scale=0.125),
                                  reads=[R_ps[sp_]], writes=[R_PT[pt_]])
                            for j in range(2):
                                c = grp * 2 + j
                                kb.op("pe", lambda pe, c=c, j=j: pe.matmul(p_o[po][:], lhsT=VA[:, c, :], rhs=PT[pt_][:, j, :],
                                                                          start=(c == 0), stop=(c == NC - 1)),
                                      reads=[R_VA, R_PT[pt_]], writes=[R_po[po]], disjoint=True)
                        kb.op("dve", lambda v: v.reciprocal(out=rec[64:128, :], in_=p_o[po][64:128, :]), reads=[R_po[po]], writes=[R_rec])
                        kb.op("dve", lambda v: v.tensor_tensor(out=on[po][:], in0=p_o[po][0:64, :], in1=rec[64:128, :], op=ALU.mult),
                              reads=[R_po[po], R_rec], writes=[R_on[po]])
                        kb.dma(ds_on[po], OT_d[(h % 2) * 64:(h % 2) * 64 + 64, h // 2, qt * 512:(qt + 1) * 512], on[po][:], reads=[R_on[po]], writes=[R_OT])
            else:
                KTp = ph.sb([64, T + 512], BF16, "KTp"); R_KTs = Res(); ds_KT = kb.dsem("KT")
                VA = ph.sb([128, NC + 4, 128], BF16, "VAp"); R_VA = Res(); ds_VA = kb.dsem("VA")
                QT1 = ph.sb([64, T], BF16, "QT"); QT = [QT1, QT1]; R_q1 = Res(); R_QTs = [R_q1, R_q1]; ds_q1 = kb.dsem("QT"); ds_QT = [ds_q1, ds_q1]
                Mt = ph.sb([128, 15, 64], F32, "Mt"); R_M = Res(); ds_M = kb.dsem("M")
                mc = ph.sb([128, 2, 64], F32, "mc"); R_mc = Res(); ds_mc = kb.dsem("mc")
                Bfull = ph.sb([128, 6, 256], F32, "Bfull"); R_Bf = Res()
                rv = ph.sb([128, 4, 1536], BF16, "rv"); R_rv = Res(); ds_rv = kb.dsem("rv")
                ngt = ph.sb([128, 1536], F32, "ngt"); R_ngt = Res()
                Bt = ph.sb([128, 4, 1536], F32, "Bt"); R_Bt = Res()
                p_s = [ph.ps([128, 6, 256], F32, "ps") for _ in range(2)]; R_ps = [Res(), Res()]
                p_o = [ph.ps([128, 256], F32, "po") for _ in range(2)]; R_po = [Res(), Res()]
                sbt1 = ph.sb([128, 1536], F32, "sbt"); sbt = [sbt1, sbt1]; R_sb1 = Res(); R_sbt = [R_sb1, R_sb1]
                PT = [ph.sb([128, 6, 256], BF16, "PT") for _ in range(2)]; R_PT = [Res(), Res()]
                rec = ph.sb([128, 256], F32, "rec"); R_rec = Res()
                on = [ph.sb([64, 256], BF16, "on") for _ in range(2)]; R_on = [Res(), Res()]; ds_on = [kb.dsem("on"), kb.dsem("on")]
                kb.dma(ds_mc, mc[:], na_mc.rearrange("a p q -> p a q"), writes=[R_mc])
                kb.dma(ds_rv, rv[:], na_rv.rearrange("a p n -> p a n"), writes=[R_rv])
                kb.op("pool", lambda v: v.memset(KTp[:, 0:256], 0.0), writes=[R_KTs])
                kb.op("pool", lambda v: v.memset(KTp[:, 256 + T:512 + T], 0.0), writes=[R_KTs], disjoint=True)
                for c0 in range(0, NC + 4, 32):
                    c1 = min(NC + 4, c0 + 32)
                    kb.op("pool", lambda v, c0=c0, c1=c1: v.memset(VA[:, c0:c1, :], 1.0), writes=[R_VA], disjoint=(c0 > 0))
                kb.op("pool", lambda v: v.memset(VA[:, 0:2, 0:64], 0.0), writes=[R_VA])
                kb.op("pool", lambda v: v.memset(VA[:, NC + 2:NC + 4, 0:64], 0.0), writes=[R_VA])
                bi = 0
                for h in range(hg, hg + hstep if '2' in PH else hg):
                    hs = h % 2
                    for c0 in range(0, T, 4096):
                        c1 = min(T, c0 + 4096)
                        kb.dma(ds_KT, KTp[:, 256 + c0:256 + c1], KT_d[:, h, c0:c1], reads=[R_KT], writes=[R_KTs], disjoint=(c0 > 0))
                    for c0 in range(0, NC, 16):
                        c1 = min(NC, c0 + 16)
                        kb.dma(ds_VA, VA[:, 2 + c0:2 + c1, 0:64], V_d[c0 * 128:c1 * 128, h * 64:(h + 1) * 64].rearrange("(c p) d -> p c d", p=128),
                               reads=[R_V], writes=[R_VA], disjoint=(c0 > 0))
                    for c0 in range(0, T, 4096):
                        c1 = min(T, c0 + 4096)
                        kb.dma(ds_QT[hs], QT[hs][:, c0:c1], QT_d[:, h, c0:c1], reads=[R_QT], writes=[R_QTs[hs]], disjoint=(c0 > 0))
                    for a in range(2):
                        kb.dma(ds_M, Mt[a * 64:(a + 1) * 64, :, :], na_m[lj, h].rearrange("r k q -> k r q"), writes=[R_M], disjoint=(a == 1))
                    kb.op("dve", lambda v: v.tensor_tensor(out=Mt[:], in0=Mt[:], in1=mc[:, 0:1, :].broadcast_to([128, 15, 64]), op=ALU.mult),
                          reads=[R_M, R_mc], writes=[R_M])
                    kb.op("dve", lambda v: v.tensor_tensor(out=Mt[:], in0=Mt[:], in1=mc[:, 1:2, :].broadcast_to([128, 15, 64]), op=ALU.add),
                          reads=[R_M, R_mc], writes=[R_M])
                    k = 0
                    for c in range(6):
                        for a in range(2):
                            for i in range(4):
                                dr = 2 * c + a - i + 3
                                eng = "pool" if k % 2 == 0 else "dve"
                                k += 1
                                kb.op(eng, lambda v, c=c, a=a, i=i, dr=dr: v.tensor_copy(
                                    out=Bfull[a * 64:(a + 1) * 64, c, i * 64:(i + 1) * 64], in_=Mt[a * 64:(a + 1) * 64, dr, :]),
                                    reads=[R_M], writes=[R_Bf], disjoint=True)
                    bff = Bfull[:].rearrange("p c q -> p (c q)")
                    for ty in range(4):
                        kb.op("dve", lambda v, ty=ty: v.tensor_tensor(out=Bt[:, ty, :], in0=bff, in1=rv[:, ty, :], op=ALU.mult),
                              reads=[R_Bf, R_rv], writes=[R_Bt], disjoint=True)
                        kb.op("dve", lambda v, ty=ty: v.tensor_scalar(out=ngt[:], in0=rv[:, ty, :], scalar1=30000.0, scalar2=-30000.0, op0=ALU.mult, op1=ALU.add),
                              reads=[R_rv], writes=[R_ngt])
                        kb.op("dve", lambda v, ty=ty: v.tensor_tensor(out=Bt[:, ty, :], in0=Bt[:, ty, :], in1=ngt[:], op=ALU.add),
                              reads=[R_Bt, R_ngt], writes=[R_Bt], disjoint=True)
                    for j in range(NB):
                        s_ = bi % 2
                        bi += 1
                        ty = 1 if j == 0 else (2 if j == NB // 2 - 1 else (3 if j == NB - 1 else 0))
                        for c in range(6):
                            k0 = (2 * j + c) * 128
                            kb.op("pe", lambda pe, c=c, k0=k0: pe.matmul(p_s[s_][:, c, :], lhsT=KTp[:, k0:k0 + 128],
                                                                        rhs=QT[hs][:, j * 256:(j + 1) * 256], start=True, stop=True),
                                  reads=[R_KTs, R_QTs[hs]], writes=[R_ps[s_]], disjoint=True)
                        kb.op("dve", lambda v: v.scalar_tensor_tensor(out=sbt[s_][:], in0=p_s[s_][:].rearrange("p c q -> p (c q)"), scalar=0.125,
                                                                      in1=Bt[:, ty, :], op0=ALU.mult, op1=ALU.add),
                              reads=[R_ps[s_], R_Bt], writes=[R_sbt[s_]])
                        kb.op("act", lambda a_: a_.activation(out=PT[s_][:].rearrange("p c q -> p (c q)"), in_=sbt[s_][:], func=AF.Exp),
                              reads=[R_sbt[s_]], writes=[R_PT[s_]])
                        for c in range(6):
                            kb.op("pe", lambda pe, c=c: pe.matmul(p_o[s_][:], lhsT=VA[:, 2 * j + c, :], rhs=PT[s_][:, c, :],
                                                                  start=(c == 0), stop=(c == 5)),
                                  reads=[R_VA, R_PT[s_]], writes=[R_po[s_]], disjoint=True)
                        kb.op("dve", lambda v: v.reciprocal(out=rec[64:128, :], in_=p_o[s_][64:128, :]), reads=[R_po[s_]], writes=[R_rec])
                        kb.op("dve", lambda v: v.tensor_tensor(out=on[s_][:], in0=p_o[s_][0:64, :], in1=rec[64:128, :], op=ALU.mult),
                              reads=[R_po[s_], R_rec], writes=[R_on[s_]])
                        kb.dma(ds_on[s_], OT_d[(h % 2) * 64:(h % 2) * 64 + 64, h // 2, j * 256:(j + 1) * 256], on[s_][:], reads=[R_on[s_]], writes=[R_OT])
            ph.close()

        for sub3 in range(0, NT, 32):
            ph = Phase()
            wo = ph.sb([128, 8, D], BF16, "wo"); R_wo = Res()
            stage = [ph.sb([128, 1024], F32, "stg") for _ in range(2)]; R_stage = [Res(), Res()]
            ds_stage = [kb.dsem("stg"), kb.dsem("stg")]
            load_w_bf16(ph, (gqa_w_o if is_gqa else na_w_o)[lj], D, wo, R_wo, stage, R_stage, ds_stage)
            wr = ph.sb([128, 8, E], F32, "wr"); R_tab = Res(); ds_tab = kb.dsem("tab")
            kb.dma(ds_tab, wr[:], router_w[li].rearrange("(k p) e -> p k e", p=128), writes=[R_tab])
            brt = ph.sb([128, E], F32, "brt")
            bcast_load(ds_tab, brt[:], router_b[li:li + 1, :], E, R_tab)
            g1 = ph.sb([128, D], F32, "g1"); b1 = ph.sb([128, D], F32, "b1")
            bcast_load(ds_tab, g1[:], ln1_g[li:li + 1, :], D, R_tab)
            bcast_load(ds_tab, b1[:], ln1_b[li:li + 1, :], D, R_tab)
            bdn = ph.sb([E, D], F32, "bdn")
            kb.dma(ds_tab, bdn[:], b_dn[li], writes=[R_tab])
            OTt = [ph.sb([128, 8, 128], BF16, "OTt") for _ in range(2)]; R_OTt = [Res(), Res()]; ds_OTt = [kb.dsem("OTt"), kb.dsem("OTt")]
            xt = [ph.sb([128, D], F32, "xt") for _ in range(2)]; R_xt = [Res(), Res()]; ds_xt = [kb.dsem("xt"), kb.dsem("xt")]
            z = ph.sb([128, D], F32, "z"); R_z = Res()
            x1 = [ph.sb([128, D], F32, "x1") for _ in range(2)]; R_x1t = [Res(), Res()]; ds_x1 = [kb.dsem("x1"), kb.dsem("x1")]
            x1b = ph.sb([128, D], BF16, "x1b"); R_x1b = Res()
            x1T = ph.sb([128, 8, 128], BF16, "x1T"); R_x1Ts = Res(); ds_x1T = kb.dsem("x1T")
            x1Tf = ph.sb([128, 8, 128], F32, "x1Tf"); R_x1Tf = Res()
            lg = ph.sb([128, E], F32, "lg"); R_lg = Res()
            m8 = ph.sb([128, 8], F32, "m8"); msk = ph.sb([128, E], F32, "msk"); ex = ph.sb([128, E], F32, "ex")
            nmx = ph.sb([128, 1], F32, "nmx"); ssm = ph.sb([128, 1], F32, "ssm"); R_r = Res()
            gt = [ph.sb([128, E], F32, "gt") for _ in range(2)]; R_gt = [Res(), Res()]; ds_gt = [kb.dsem("gt"), kb.dsem("gt")]
            gT = ph.sb([E, 128], F32, "gT"); R_gT = Res()
            ai = [ph.sb([128, D], F32, "ai") for _ in range(2)]; R_ai = [Res(), Res()]; ds_ai = [kb.dsem("ai"), kb.dsem("ai")]
            st6 = ph.sb([128, 2, 6], F32, "st6"); mv = ph.sb([128, 2], F32, "mv"); rstd = ph.sb([128, 1], F32, "rstd"); R_lnt = Res()
            p_y = ph.ps([128, D], F32, "py"); R_py = Res()
            p_xb = ph.ps([128, 8, 128], BF16, "pxb"); R_pxb = Res()
            p_xf = ph.ps([128, 8, 128], F32, "pxf"); R_pxf = Res()
            p_l = ph.ps([128, 512], F32, "pl"); R_pl = Res()
            p_a = ph.ps([128, D], F32, "pa"); R_pa = Res()
            for t in range(sub3, min(NT, sub3 + 32) if '3' in PH else 0):
                s = t % 2
                t0 = t * 128
                kb.dma(ds_OTt[s], OTt[s][:], OT_d[:, :, t0:t0 + 128], reads=[R_OT], writes=[R_OTt[s]], disjoint=False)
                kb.dma(ds_xt[s], xt[s][:], src_x[t0:t0 + 128, :], reads=[R_src], writes=[R_xt[s]], disjoint=False)
                for nh in range(2):
                    for kc in range(8):
                        kb.op("pe", lambda pe, nh=nh, kc=kc: pe.matmul(p_y[:, nh * 512:(nh + 1) * 512], lhsT=OTt[s][:, kc, :],
                                                                      rhs=wo[:, kc, nh * 512:(nh + 1) * 512], start=(kc == 0), stop=(kc == 7)),
                              reads=[R_OTt[s], R_wo], writes=[R_py], disjoint=True)
                kb.op("dve", lambda v: v.scalar_tensor_tensor(out=z[:], in0=xt[s][:], scalar=float(alpha), in1=p_y[:], op0=ALU.mult, op1=ALU.add),
                      reads=[R_xt[s], R_py], writes=[R_z])
                layer_norm(z, R_z, x1[s], R_x1t[s], g1, b1, R_tab, (st6, mv, rstd, R_lnt))
                kb.dma(ds_x1[s], x1_d[t0:t0 + 128, :], x1[s][:], reads=[R_x1t[s]], writes=[R_x1])
                kb.op("act", lambda a: a.activation(func=AF.Copy, out=x1b[:], in_=x1[s][:]), reads=[R_x1t[s]], writes=[R_x1b])
                for kc in range(8):
                    kb.op("pe", lambda pe, kc=kc: pe.transpose(out=p_xb[:, kc, :], in_=x1b[:, kc * 128:(kc + 1) * 128], identity=ident_b[:]),
                          reads=[R_x1b, R_const], writes=[R_pxb], disjoint=True)
                kb.op("act", lambda a: a.activation(func=AF.Copy, out=x1T[:], in_=p_xb[:]), reads=[R_pxb], writes=[R_x1Ts])
                kb.dma(ds_x1T, x1T_d[:, :, t0:t0 + 128], x1T[:], reads=[R_x1Ts], writes=[R_x1T])
                for kc in range(8):
                    kb.op("pe", lambda pe, kc=kc: pe.transpose(out=p_xf[:, kc, :], in_=x1[s][:, kc * 128:(kc + 1) * 128], identity=ident_f[:]),
                          reads=[R_x1t[s], R_const], writes=[R_pxf], disjoint=True)
                kb.op("dve", lambda v: v.tensor_copy(out=x1Tf[:], in_=p_xf[:]), reads=[R_pxf], writes=[R_x1Tf])
                for kc in range(8):
                    kb.op("pe", lambda pe, kc=kc: pe.matmul(p_l[:, 0:E], lhsT=x1Tf[:, kc, :], rhs=wr[:, kc, :], start=(kc == 0), stop=(kc == 7)),
                          reads=[R_x1Tf, R_tab], writes=[R_pl], disjoint=True)
                kb.op("dve", lambda v: v.tensor_tensor(out=lg[:], in0=p_l[:, 0:E], in1=brt[:], op=ALU.add), reads=[R_pl, R_tab], writes=[R_lg])
                kb.op("dve", lambda v: v.max(out=m8[:], in_=lg[:]), reads=[R_lg], writes=[R_r])
                kb.op("dve", lambda v: v.tensor_scalar(out=msk[:], in0=lg[:], scalar1=m8[:, 3:4], scalar2=None, op0=ALU.is_ge), reads=[R_lg, R_r], writes=[R_r], disjoint=True)
                kb.op("dve", lambda v: v.tensor_scalar(out=nmx[:], in0=m8[:, 0:1], scalar1=-1.0, scalar2=None, op0=ALU.mult), reads=[R_r], writes=[R_r], disjoint=True)
                kb.op("act", lambda a: a.activation(out=ex[:], in_=lg[:], func=AF.Exp, bias=nmx[:, 0:1], scale=1.0), reads=[R_lg, R_r], writes=[R_r], disjoint=True)
                kb.op("dve", lambda v: v.tensor_tensor(out=ex[:], in0=ex[:], in1=msk[:], op=ALU.mult), reads=[R_r], writes=[R_r])
                kb.op("dve", lambda v: v.tensor_reduce(out=ssm[:], in_=ex[:], axis=AX.X, op=ALU.add), reads=[R_r], writes=[R_r])
                kb.op("dve", lambda v: v.reciprocal(out=ssm[:], in_=ssm[:]), reads=[R_r], writes=[R_r])
                kb.op("dve", lambda v: v.tensor_scalar(out=gt[s][:], in0=ex[:], scalar1=ssm[:, 0:1], scalar2=None, op0=ALU.mult), reads=[R_r], writes=[R_gt[s]])
                kb.dma(ds_gt[s], gates_d[t0:t0 + 128, :], gt[s][:], reads=[R_gt[s]], writes=[R_gates])
                kb.op("pe", lambda pe: pe.transpose(out=p_l[0:E, 128:256], in_=gt[s][:], identity=ident_f[:]),
                      reads=[R_gt[s], R_const, R_lg], writes=[R_pl])
                kb.op("dve", lambda v: v.tensor_copy(out=gT[:], in_=p_l[0:E, 128:256]), reads=[R_pl], writes=[R_gT])
                for nh in range(2):
                    kb.op("pe", lambda pe, nh=nh: pe.matmul(p_a[:, nh * 512:(nh + 1) * 512], lhsT=gT[:], rhs=bdn[:, nh * 512:(nh + 1) * 512], start=True, stop=True),
                          reads=[R_gT, R_tab], writes=[R_pa], disjoint=True)
                kb.op("act", lambda a: a.activation(func=AF.Copy, out=ai[s][:], in_=p_a[:]), reads=[R_pa], writes=[R_ai[s]])
                kb.dma(ds_ai[s], acci_d[t0:t0 + 128, :], ai[s][:], reads=[R_ai[s]], writes=[R_acci])
            ph.close()

        ph = Phase()
        sg = [ph.sb([128, 2 * D], F32, "sg") for _ in range(3)]; R_sg = [Res() for _ in range(3)]; ds_sg = [kb.dsem("sg") for _ in range(3)]
        cb = [ph.sb([128, 2, D], BF16, "cb") for _ in range(3)]; R_cb = [Res() for _ in range(3)]; ds_cb = [kb.dsem("cb") for _ in range(3)]
        engs = ["dve", "pool", "act"]
        i3 = 0
        for e in range(E if '4' in PH else 0):
            for kc in range(8):
                s = i3 % 3
                i3 += 1
                kb.dma(ds_sg[s], sg[s][:], w_gu[li, e, kc * 128:(kc + 1) * 128, :], writes=[R_sg[s]], disjoint=False)
                src = sg[s][:].rearrange("p (f two) -> p two f", two=2)
                if engs[s] == "act":
                    kb.op("act", lambda a, s=s, src=src: a.activation(func=AF.Copy, out=cb[s][:], in_=src), reads=[R_sg[s]], writes=[R_cb[s]])
                else:
                    kb.op(engs[s], lambda v, s=s, src=src: v.tensor_copy(out=cb[s][:], in_=src), reads=[R_sg[s]], writes=[R_cb[s]])
                kb.dma(ds_cb[s], wgu_b[e, :, kc * 2 * D:(kc + 1) * 2 * D], cb[s][:].rearrange("p a f -> p (a f)"),
                       reads=[R_cb[s]], writes=[R_wgu])
            for kc in range(0, 8, 2):
                s = i3 % 3
                i3 += 1
                kb.dma(ds_sg[s], sg[s][:].rearrange("p (a f) -> p a f", a=2),
                       w_dn[li, e, kc * 128:(kc + 2) * 128, :].rearrange("(a p) f -> p a f", p=128), writes=[R_sg[s]], disjoint=False)
                src = sg[s][:].rearrange("p (a f) -> p a f", a=2)
                if engs[s] == "act":
                    kb.op("act", lambda a, s=s, src=src: a.activation(func=AF.Copy, out=cb[s][:], in_=src), reads=[R_sg[s]], writes=[R_cb[s]])
                else:
                    kb.op(engs[s], lambda v, s=s, src=src: v.tensor_copy(out=cb[s][:], in_=src), reads=[R_sg[s]], writes=[R_cb[s]])
                kb.dma(ds_cb[s], wd_b[e, :, kc * D:(kc + 2) * D], cb[s][:].rearrange("p a f -> p (a f)"),
                       reads=[R_cb[s]], writes=[R_wd])
        ph.close()

        for sub5 in range(0, T // BLK, 8):
            ph = Phase()
            Wg = [ph.sb([128, 8, 2, D], BF16, "Wg") for _ in range(2)]; R_Wg = [Res(), Res()]; ds_Wg = [kb.dsem("Wg"), kb.dsem("Wg")]
            Wd = [ph.sb([128, 8, D], BF16, "Wd") for _ in range(2)]; R_Wd = [Res(), Res()]; ds_Wd = [kb.dsem("Wd"), kb.dsem("Wd")]
            bgu = ph.sb([128, E, 8, 2], F32, "bgu"); R_tab = Res(); ds_tab = kb.dsem("tab")
            for e in range(E):
                kb.dma(ds_tab, bgu[:, e, :, :], b_gu[li, e].rearrange("(c p two) -> p c two", p=128, two=2), writes=[R_tab])
            g2 = ph.sb([128, D], F32, "g2"); b2 = ph.sb([128, D], F32, "b2")
            bcast_load(ds_tab, g2[:], ln2_g[li:li + 1, :], D, R_tab)
            bcast_load(ds_tab, b2[:], ln2_b[li:li + 1, :], D, R_tab)
            NTB = BLK // 128
            xTb = ph.sb([128, 8, BLK], BF16, "xTb"); R_xTb = Res(); ds_xTb = kb.dsem("xTb")
            gts = ph.sb([128, NTB, E], F32, "gts"); R_gts = Res(); ds_gts = kb.dsem("gts")
            acc = ph.sb([128, NTB, D], F32, "acc"); R_acc = Res(); ds_acc = kb.dsem("acc")
            AT = ph.sb([128, 8, BLK], BF16, "AT"); R_AT = Res()
            gp = [ph.sb([128, BLK], F32, "gp") for _ in range(2)]; R_gp = [Res(), Res()]
            sgm = [ph.sb([128, BLK], F32, "sgm") for _ in range(2)]; R_sgm = [Res(), Res()]
            up = [ph.sb([128, BLK], F32, "up") for _ in range(2)]; R_up = [Res(), Res()]
            x1t = ph.sb([128, D], F32, "x1t"); R_x1tt = Res(); ds_x1t = kb.dsem("x1t")
            xo = [ph.sb([128, D], F32, "xo") for _ in range(2)]; R_xo = [Res(), Res()]; ds_xo = [kb.dsem("xo"), kb.dsem("xo")]
            st6 = ph.sb([128, 2, 6], F32, "st6"); mv = ph.sb([128, 2], F32, "mv"); rstd = ph.sb([128, 1], F32, "rstd"); R_lnt = Res()
            p_g = [ph.ps([128, BLK], F32, "pg") for _ in range(2)]; R_pg = [Res(), Res()]
            p_u = [ph.ps([128, BLK], F32, "pu") for _ in range(2)]; R_pu = [Res(), Res()]
            p_y = [ph.ps([128, 512], F32, "py") for _ in range(2)]; R_pyy = [Res(), Res()]
            dst_x = y_out if li == DEPTH - 1 else xres
            R_dst = R_y if li == DEPTH - 1 else R_xres
            wi = 0; fi = 0; yi = 0; oi = 0
            for tb in range(sub5, min(T // BLK, sub5 + 8) if '5' in PH else sub5):
                b0 = tb * BLK
                kb.dma(ds_xTb, xTb[:], x1T_d[:, :, b0:b0 + BLK], reads=[R_x1T], writes=[R_xTb], disjoint=False)
                kb.dma(ds_gts, gts[:], gates_d[b0:b0 + BLK, :].rearrange("(n p) e -> p n e", p=128), reads=[R_gates], writes=[R_gts], disjoint=False)
                kb.dma(ds_acc, acc[:], acci_d[b0:b0 + BLK, :].rearrange("(n p) d -> p n d", p=128), reads=[R_acci], writes=[R_acc], disjoint=False)
                for e in range(E):
                    w = wi % 2
                    wi += 1
                    kb.dma(ds_Wg[w], Wg[w][:].rearrange("p k a f -> p (k a f)"), wgu_b[e], reads=[R_wgu], writes=[R_Wg[w]], disjoint=False)
                    kb.dma(ds_Wd[w], Wd[w][:].rearrange("p k f -> p (k f)"), wd_b[e], reads=[R_wd], writes=[R_Wd[w]], disjoint=False)
                    for fc in range(8):
                        f = fi % 2
                        fi += 1
                        for kc in range(8):
                            kb.op("pe", lambda pe, kc=kc, fc=fc: pe.matmul(p_g[f][:], lhsT=Wg[w][:, kc, 0, fc * 128:(fc + 1) * 128], rhs=xTb[:, kc, :],
                                                                          start=(kc == 0), stop=(kc == 7)),
                                  reads=[R_Wg[w], R_xTb], writes=[R_pg[f]], disjoint=True)
                        for kc in range(8):
                            kb.op("pe", lambda pe, kc=kc, fc=fc: pe.matmul(p_u[f][:], lhsT=Wg[w][:, kc, 1, fc * 128:(fc + 1) * 128], rhs=xTb[:, kc, :],
                                                                          start=(kc == 0), stop=(kc == 7)),
                                  reads=[R_Wg[w], R_xTb], writes=[R_pu[f]], disjoint=True)
                        kb.op("dve", lambda v, fc=fc: v.tensor_scalar(out=gp[f][:], in0=p_g[f][:], scalar1=bgu[:, e, fc, 0:1], scalar2=7.0, op0=ALU.add, op1=ALU.min),
                              reads=[R_pg[f], R_tab], writes=[R_gp[f]])
                        kb.op("act", lambda a: a.activation(out=sgm[f][:], in_=gp[f][:], func=AF.Sigmoid, scale=1.702), reads=[R_gp[f]], writes=[R_sgm[f]])
                        kb.op("dve", lambda v, fc=fc: v.tensor_scalar(out=up[f][:], in0=p_u[f][:], scalar1=bgu[:, e, fc, 1:2], scalar2=7.0, op0=ALU.add, op1=ALU.min),
                              reads=[R_pu[f], R_tab], writes=[R_up[f]])
                        kb.op("dve", lambda v: v.tensor_scalar(out=up[f][:], in0=up[f][:], scalar1=-7.0, scalar2=1.0, op0=ALU.max, op1=ALU.add),
                              reads=[R_up[f]], writes=[R_up[f]])
                        kb.op("pool", lambda v: v.tensor_tensor(out=gp[f][:], in0=gp[f][:], in1=sgm[f][:], op=ALU.mult), reads=[R_gp[f], R_sgm[f]], writes=[R_gp[f]])
                        kb.op("pool", lambda v, fc=fc: v.tensor_tensor(out=AT[:, fc, :], in0=gp[f][:], in1=up[f][:], op=ALU.mult),
                              reads=[R_gp[f], R_up[f]], writes=[R_AT], disjoint=True)
                    for n in range(NTB):
                        for dh in range(2):
                            y_ = yi % 2
                            yi += 1
                            for fc in range(8):
                                kb.op("pe", lambda pe, fc=fc, n=n, dh=dh: pe.matmul(p_y[y_][:], lhsT=AT[:, fc, n * 128:(n + 1) * 128],
                                                                                  rhs=Wd[w][:, fc, dh * 512:(dh + 1) * 512], start=(fc == 0), stop=(fc == 7)),
                                      reads=[R_AT, R_Wd[w]], writes=[R_pyy[y_]], disjoint=True)
                            kb.op("dve", lambda v, n=n, dh=dh: v.scalar_tensor_tensor(
                                out=acc[:, n, dh * 512:(dh + 1) * 512], in0=p_y[y_][:], scalar=gts[:, n, e:e + 1],
                                in1=acc[:, n, dh * 512:(dh + 1) * 512], op0=ALU.mult, op1=ALU.add),
                                reads=[R_pyy[y_], R_gts, R_acc], writes=[R_acc])
                for n in range(NTB):
                    o = oi % 2
                    oi += 1
                    t0 = b0 + n * 128
                    kb.dma(ds_x1t, x1t[:], x1_d[t0:t0 + 128, :], reads=[R_x1], writes=[R_x1tt], disjoint=False)
                    kb.op("dve", lambda v, n=n: v.scalar_tensor_tensor(out=acc[:, n, :], in0=x1t[:], scalar=float(alpha), in1=acc[:, n, :], op0=ALU.mult, op1=ALU.add),
                          reads=[R_x1tt, R_acc], writes=[R_acc])
                    R_accn = R_acc
                    layer_norm(acc[:, n, :], R_accn, xo[o], R_xo[o], g2, b2, R_tab, (st6, mv, rstd, R_lnt))
                    kb.dma(ds_xo[o], dst_x[t0:t0 + 128, :], xo[o][:], reads=[R_xo[o]], writes=[R_dst])
            ph.close()

    kb.barrier()
    kb.final_wait()
    pc.st.close()
    return nc


def _rope_tables(T):
    t = np.arange(T)
    row = (t // GW).astype(np.float32)
    col = (t % GW).astype(np.float32)
    freqs = (np.float32(10000.0) ** (-np.arange(0, 32, 2, dtype=np.float32) / np.float32(32))).astype(np.float32)
    ar = (row[:, None] * freqs).astype(np.float32)
    ac = (col[:, None] * freqs).astype(np.float32)
    cr, sr, cc, sc = np.cos(ar), np.sin(ar), np.cos(ac), np.sin(ac)
    C = np.concatenate([cr, cr, cc, cc], axis=1).astype(np.float32)
    S = np.concatenate([-sr, sr, -sc, sc], axis=1).astype(np.float32)
    return C, S


def _na_tables(n_rows_valid, T):
    qc = np.arange(64)
    cs = np.clip(qc - 8, 0, 48)
    kc = np.arange(64)
    m = ((kc[:, None] >= cs[None, :]) & (kc[:, None] < cs[None, :] + 16)).astype(np.float32)
    mc = np.stack([np.concatenate([m, m], 0), np.concatenate([(m - 1) * 30000.0] * 2, 0)], 0).astype(np.float32)
    def rv(lo_fn):
        v = np.zeros((128, 6, 256), np.float32)
        for c in range(6):
            for a in range(2):
                kr = 2 * c + a
                for i in range(4):
                    lo, hi = lo_fn(i)
                    if lo <= kr < hi:
                        v[a * 64:(a + 1) * 64, c, i * 64:(i + 1) * 64] = 1.0
        return v
    interior = rv(lambda i: (i, i + 8))
    first = rv(lambda i: (4, 12))
    last = rv(lambda i: (0, 8))
    last_blk = n_rows_valid // 4 - 1
    NBt = T // 256
    tabs = [interior, first, last if last_blk == NBt // 2 - 1 else interior, last if last_blk == NBt - 1 else interior]
    out = np.zeros((4, 128, 1536), np.float32)
    for k, tb in enumerate(tabs):
        out[k] = tb.reshape(128, 1536)
    return mc, out.astype(ml_dtypes.bfloat16)


def _na_m(rpb):
    kc = np.arange(64)[:, None]
    qc = np.arange(64)[None, :]
    idx = np.clip(kc - qc + 15, 0, 30)
    return np.ascontiguousarray(rpb[:, :, :, idx])


_CACHE = {}


def run_cores(seqs, inputs, T, E, DEPTH, alpha, li0=0):
    key = (T, E, DEPTH, float(alpha), li0 % 2)
    if key not in _CACHE:
        _CACHE[key] = build_program(T, E, DEPTH, alpha, li0)
    nc = _CACHE[key]
    C, S = _rope_tables(T)
    na_m = _na_m(np.asarray(inputs["na_rpb"], np.float32))
    shared = {
        "gqa_w_qkv": inputs["gqa_w_qkv"], "gqa_q_norm": inputs["gqa_q_norm"], "gqa_k_norm": inputs["gqa_k_norm"],
        "gqa_w_o": inputs["gqa_w_o"], "na_w_qkv": inputs["na_w_qkv"], "na_m": na_m, "na_w_o": inputs["na_w_o"],
        "ln1_g": inputs["ln1_g"], "ln1_b": inputs["ln1_b"], "ln2_g": inputs["ln2_g"], "ln2_b": inputs["ln2_b"],
        "router_w": inputs["router_w"], "router_b": inputs["router_b"],
        "exp_w_gu": inputs["exp_w_gu"], "exp_b_gu": inputs["exp_b_gu"],
        "exp_w_down": inputs["exp_w_down"], "exp_b_down": inputs["exp_b_down"],
        "rope_c": C, "rope_s": S, "ident_f": np.eye(128, dtype=np.float32),
    }
    shared = {k: np.ascontiguousarray(np.asarray(v, np.float32)) for k, v in shared.items()}
    in_maps = []
    for xs in seqs:
        Sx = xs.shape[0]
        xp = np.zeros((T, D), np.float32)
        xp[:Sx] = xs
        tv = (np.arange(T) < Sx).astype(np.float32).reshape(T // 128, 128).T.copy()
        mc, rvt = _na_tables(Sx // GW, T)
        m = dict(shared)
        m.update({"x": xp, "tv": np.ascontiguousarray(tv), "na_rv": rvt, "na_mc": mc})
        in_maps.append(m)
    res = run_bass_kernel_spmd(nc, in_maps, core_ids=list(range(len(seqs))))
    return [np.asarray(res.results[i]["y"])[: seqs[i].shape[0]] for i in range(len(seqs))]


LAYERS_PER_LAUNCH = 1


def _slice_layers(inputs, li0, n):
    out = {}
    gq = [l // 2 for l in range(li0, li0 + n) if l % 2 == 0]
    na = [l // 2 for l in range(li0, li0 + n) if l % 2 == 1]
    gs = slice(gq[0], gq[-1] + 1) if gq else slice(0, 1)
    ns = slice(na[0], na[-1] + 1) if na else slice(0, 1)
    for k in ("gqa_w_qkv", "gqa_q_norm", "gqa_k_norm", "gqa_w_o"):
        out[k] = np.asarray(inputs[k])[gs]
    for k in ("na_w_qkv", "na_rpb", "na_w_o"):
        out[k] = np.asarray(inputs[k])[ns]
    for k in ("ln1_g", "ln1_b", "ln2_g", "ln2_b", "router_w", "router_b",
              "exp_w_gu", "exp_b_gu", "exp_w_down", "exp_b_down"):
        out[k] = np.asarray(inputs[k])[li0:li0 + n]
    return out


def kernel(**inputs):
    xp = np.asarray(inputs["x_prompt"], np.float32)
    xs = np.asarray(inputs["x_sample"], np.float32)
    DEPTH = 4
    alpha = (2 * DEPTH) ** 0.25
    seqs = [xp[0], xs[0], xs[1]]
    for li0 in range(0, DEPTH, LAYERS_PER_LAUNCH):
        seqs = run_cores(seqs, _slice_layers(inputs, li0, LAYERS_PER_LAUNCH), 16384, 32, LAYERS_PER_LAUNCH, alpha, li0)
    y_prompt = seqs[0][None].astype(np.float32)
    y_sample = np.stack([seqs[1], seqs[2]], 0).astype(np.float32)
    return (y_prompt, y_sample)
```

```python
import contextlib
import os
PH = os.environ.get('KPH', '12345')
import numpy as np
import ml_dtypes
import concourse.bass as bass
import concourse.mybir as mybir
from concourse.bass_utils import run_bass_kernel_spmd

F32 = mybir.dt.float32
BF16 = mybir.dt.bfloat16
AF = mybir.ActivationFunctionType
ALU = mybir.AluOpType
AX = mybir.AxisListType

D = 1024
GW = 64
HD = 64
NH = 16
NKV = 4
LN_EPS = 1e-5
RMS_EPS = 1e-6
NEG = -30000.0


class Res:
    __slots__ = ("w", "r", "name")

    def __init__(self, name=""):
        self.w = {}
        self.r = {}
        self.name = name


class DSem:
    def __init__(self, sem, name):
        self.sem = sem
        self.total = 0
        self.name = name


class KB:
    def __init__(self, nc):
        self.nc = nc
        self.eng = {"pe": nc.tensor, "act": nc.scalar, "dve": nc.vector, "pool": nc.gpsimd, "sp": nc.sync}
        self.esem = {}
        for k in ("pe", "act", "dve", "pool"):
            self.esem[k] = nc.alloc_semaphore(name=f"prog_{k}")
        self.cnt = {k: 0 for k in self.esem}
        self.known = {k: {} for k in self.eng}
        self.dsems = []
        self.free_ds = []
        self.live_ds = []
        self.uid = 0

    def dsem(self, name):
        if self.free_ds:
            d = self.free_ds.pop()
        else:
            d = DSem(self.nc.alloc_semaphore(name=f"d_{name}_{len(self.dsems)}"), name)
            self.dsems.append(d)
        self.live_ds.append(d)
        return d

    def release_ds(self, keep=()):
        for d in self.live_ds:
            if d not in keep:
                self.free_ds.append(d)
        self.live_ds = [d for d in self.live_ds if d in keep]

    def _wait(self, e, key, sem, val):
        if val <= 0:
            return
        if key == e:
            return
        if self.known[e].get(key, 0) >= val:
            return
        self.eng[e].wait_ge(sem, val)
        self.known[e][key] = val

    def _deps(self, e, reads, writes, disjoint):
        for R in reads:
            for key, (sem, val) in R.w.items():
                if key == e and e != "pe":
                    if self.known[e].get("self", 0) < val:
                        self.eng[e].wait_ge(sem, val)
                        self.known[e]["self"] = val
                    continue
                self._wait(e, key, sem, val)
        for R in writes:
            for key, (sem, val) in R.r.items():
                self._wait(e, key, sem, val)
            if not disjoint:
                for key, (sem, val) in R.w.items():
                    self._wait(e, key, sem, val)

    def _record(self, key, sem, val, reads, writes, disjoint):
        for R in reads:
            R.r[key] = (sem, val)
        for R in writes:
            if R.r or not disjoint:
                R.w = {}
                R.r = {}
            R.w[key] = (sem, val)

    def op(self, e, fn, reads=(), writes=(), disjoint=False):
        self._deps(e, reads, writes, disjoint)
        ins = fn(self.eng[e])
        self.cnt[e] += 1
        ins.then_inc(self.esem[e], 1)
        self._record(e, self.esem[e], self.cnt[e], reads, writes, disjoint)
        return ins

    def dma(self, ds, out, in_, reads=(), writes=(), q="sp", disjoint=True):
        self._deps(q, reads, writes, disjoint)
        self._wait(q, id(ds), ds.sem, ds.total)
        ins = self.eng[q].dma_start(out=out, in_=in_)
        ds.total += 16
        ins.then_inc(ds.sem, 16)
        self._record(id(ds), ds.sem, ds.total, reads, writes, disjoint)

    def barrier(self):
        evs = [(k, self.esem[k], self.cnt[k]) for k in self.esem]
        evs += [(id(d), d.sem, d.total) for d in self.dsems]
        for e in self.eng:
            for key, sem, val in evs:
                self._wait(e, key, sem, val)

    def final_wait(self):
        for d in self.dsems:
            self._wait("sp", id(d), d.sem, d.total)


def build_program(T, E, DEPTH, alpha, li0=0):
    NT = T // 128
    NC = T // 128
    NB = T // 256
    n_gqa = sum(1 for l in range(li0, li0 + DEPTH) if l % 2 == 0)
    n_na = DEPTH - n_gqa
    BLK = 512
    nc = bass.Bass("TRN2", target_bir_lowering=False)
    kb = KB(nc)

    def din(name, shape, dt=F32):
        return nc.dram_tensor(name, list(shape), dt, kind="ExternalInput").ap()

    def dscr(name, shape, dt):
        return nc.dram_tensor(name, list(shape), dt, kind="Internal").ap()

    x_in = din("x", [T, D])
    gqa_w_qkv = din("gqa_w_qkv", [max(n_gqa, 1), D, 1536])
    gqa_qn = din("gqa_q_norm", [max(n_gqa, 1), HD])
    gqa_kn = din("gqa_k_norm", [max(n_gqa, 1), HD])
    gqa_w_o = din("gqa_w_o", [max(n_gqa, 1), D, D])
    na_w_qkv = din("na_w_qkv", [max(n_na, 1), D, 3 * D])
    na_m = din("na_m", [max(n_na, 1), NH, 15, 64, 64])
    na_w_o = din("na_w_o", [max(n_na, 1), D, D])
    ln1_g = din("ln1_g", [DEPTH, D]); ln1_b = din("ln1_b", [DEPTH, D])
    ln2_g = din("ln2_g", [DEPTH, D]); ln2_b = din("ln2_b", [DEPTH, D])
    router_w = din("router_w", [DEPTH, D, E]); router_b = din("router_b", [DEPTH, E])
    w_gu = din("exp_w_gu", [DEPTH, E, D, 2 * D]); b_gu = din("exp_b_gu", [DEPTH, E, 2 * D])
    w_dn = din("exp_w_down", [DEPTH, E, D, D]); b_dn = din("exp_b_down", [DEPTH, E, D])
    rope_c = din("rope_c", [T, 64]); rope_s = din("rope_s", [T, 64])
    tv_in = din("tv", [128, NC])
    na_rv = din("na_rv", [4, 128, 6 * 256], BF16)
    na_mc = din("na_mc", [2, 128, 64])
    ident_f_in = din("ident_f", [128, 128])
    y_out = nc.dram_tensor("y", [T, D], F32, kind="ExternalOutput").ap()

    xres = dscr("xres", [T, D], F32)
    x1_d = dscr("x1_d", [T, D], F32)
    QT_d = dscr("QT_d", [HD, NH, T], BF16)
    KT_d = dscr("KT_d", [HD, NH, T], BF16)
    V_d = dscr("V_d", [T, NH * HD], BF16)
    OT_d = dscr("OT_d", [128, 8, T], BF16)
    x1T_d = dscr("x1T_d", [128, 8, T], BF16)
    gates_d = dscr("gates_d", [T, E], F32)
    acci_d = dscr("acci_d", [T, D], F32)
    wgu_b = dscr("wgu_b", [E, 128, 8 * 2 * D], BF16)
    wd_b = dscr("wd_b", [E, 128, 8 * D], BF16)

    R_xres = Res("xres"); R_x1 = Res("x1"); R_QT = Res("QT"); R_KT = Res("KT"); R_V = Res("V")
    R_OT = Res("OT"); R_x1T = Res("x1T"); R_gates = Res("gates"); R_acci = Res("acci")
    R_wgu = Res("wgu"); R_wd = Res("wd"); R_y = Res("y")

    names = [0]

    def uname(p):
        names[0] += 1
        return f"{p}_{names[0]}"

    class Phase:
        def __init__(self):
            self.st = contextlib.ExitStack()

        def sb(self, shape, dt, name="t"):
            return self.st.enter_context(nc.sbuf_tensor(uname(name), list(shape), dt))

        def ps(self, shape, dt, name="p"):
            return self.st.enter_context(nc.psum_tensor(uname(name), list(shape), dt))

        def close(self):
            kb.barrier()
            kb.release_ds(keep=(ds_c,))
            self.st.close()

    pc = Phase()
    ident_f = pc.sb([128, 128], F32, "identf")
    ident_b = pc.sb([128, 128], BF16, "identb")
    R_const = Res("const")
    ds_c = kb.dsem("const")
    kb.dma(ds_c, ident_f[:], ident_f_in[:, :], writes=[R_const])
    kb.op("dve", lambda v: v.tensor_copy(out=ident_b[:], in_=ident_f[:]), reads=[R_const], writes=[R_const])

    def load_w_bf16(ph, w_ap, N, dst, R_dst, stage, R_stage, ds_stage):
        for kc in range(8):
            s = kc % 2
            kb.dma(ds_stage[s], stage[s][:, 0:N], w_ap[kc * 128:(kc + 1) * 128, :], writes=[R_stage[s]], disjoint=False)
            eng = "dve" if kc % 2 == 0 else "pool"
            kb.op(eng, lambda v, s=s, kc=kc: v.tensor_copy(out=dst[:, kc, :], in_=stage[s][:, 0:N]),
                  reads=[R_stage[s]], writes=[R_dst], disjoint=True)

    def bcast_load(ds, dst_ap, src_row_ap, n, R_dst):
        kb.dma(ds, dst_ap, src_row_ap.broadcast_to([128, n]), writes=[R_dst])

    def layer_norm(z, R_z, out, R_out, gt, bt, R_tab, tmp):
        st6, mv, rstd, R_t = tmp
        kb.op("dve", lambda v: v.bn_stats(out=st6[:, 0, :], in_=z[:, 0:512]), reads=[R_z], writes=[R_t])
        kb.op("dve", lambda v: v.bn_stats(out=st6[:, 1, :], in_=z[:, 512:1024]), reads=[R_z], writes=[R_t], disjoint=True)
        kb.op("dve", lambda v: v.bn_aggr(out=mv[:], in_=st6[:].rearrange("p a b -> p (a b)")), reads=[R_t], writes=[R_t])
        kb.op("dve", lambda v: v.tensor_scalar(out=rstd[:], in0=mv[:, 1:2], scalar1=LN_EPS, scalar2=None, op0=ALU.add),
              reads=[R_t], writes=[R_t])
        kb.op("act", lambda a: a.activation(out=rstd[:], in_=rstd[:], func=AF.Sqrt), reads=[R_t], writes=[R_t])
        kb.op("dve", lambda v: v.reciprocal(out=rstd[:], in_=rstd[:]), reads=[R_t], writes=[R_t])
        kb.op("dve", lambda v: v.tensor_scalar(out=z[:], in0=z[:], scalar1=mv[:, 0:1], scalar2=rstd[:, 0:1],
                                               op0=ALU.subtract, op1=ALU.mult), reads=[R_z, R_t], writes=[R_z])
        kb.op("pool", lambda v: v.tensor_tensor(out=z[:], in0=z[:], in1=gt[:], op=ALU.mult), reads=[R_z, R_tab], writes=[R_z])
        kb.op("pool", lambda v: v.tensor_tensor(out=out[:], in0=z[:], in1=bt[:], op=ALU.add), reads=[R_z, R_tab], writes=[R_out])

    for li in range(DEPTH):
        is_gqa = ((li0 + li) % 2 == 0)
        lj = sum(1 for l in range(li0, li0 + li) if (l % 2 == 0) == is_gqa)
        src_x = x_in if li == 0 else xres
        R_src = R_const if li == 0 else R_xres
        for sub1 in range(0, NT, 32):
            ph = Phase()
            NQKV = 1536 if is_gqa else 3072
            wq = ph.sb([128, 8, NQKV], BF16, "wqkv")
            R_wq = Res()
            stage = [ph.sb([128, 3072], F32, "stg") for _ in range(2)]
            R_stage = [Res(), Res()]
            ds_stage = [kb.dsem("stg"), kb.dsem("stg")]
            load_w_bf16(ph, (gqa_w_qkv if is_gqa else na_w_qkv)[lj], NQKV, wq, R_wq, stage, R_stage, ds_stage)
            xt = [ph.sb([128, D], F32, "xt") for _ in range(2)]
            R_xt = [Res(), Res()]
            ds_xt = [kb.dsem("xt"), kb.dsem("xt")]
            xb = ph.sb([128, D], BF16, "xb"); R_xb = Res()
            xT = ph.sb([128, 8, 128], BF16, "xT"); R_xT = Res()
            p_xT = ph.ps([128, 8, 128], BF16, "pxT"); R_pxT = Res()
            p_pr = ph.ps([128, 1024], F32, "ppr"); R_ppr = Res()
            p_tr = ph.ps([64, 16, 128], BF16, "ptr"); R_ptr = Res()
            qb = ph.sb([128, 16, 64], BF16, "qb"); R_qb = Res()
            qT = ph.sb([64, 16, 128], BF16, "qT"); R_qT = Res()
            ds_qT = kb.dsem("qT")
            vb = ph.sb([128, 1024], BF16, "vb"); R_vb = Res(); ds_vb = kb.dsem("vb")
            if is_gqa:
                gain = ph.sb([128, 20, 64], F32, "gain"); R_gain = Res(); ds_g = kb.dsem("gain")
                for h in range(20):
                    src = gqa_qn if h < 16 else gqa_kn
                    bcast_load(ds_g, gain[:, h, :], src[lj:lj + 1, :], 64, R_gain)
                ct = [ph.sb([128, 64], F32, "ct") for _ in range(2)]
                sn = [ph.sb([128, 64], F32, "sn") for _ in range(2)]
                R_cs = [Res(), Res()]; ds_cs = [kb.dsem("cs"), kb.dsem("cs")]
                sq = ph.sb([128, 16, 64], F32, "sq"); R_sq = Res()
                ssum = ph.sb([128, 16], F32, "ssum"); R_ss = Res()
                qn = ph.sb([128, 16, 64], F32, "qn"); R_qn = Res()
                t1 = ph.sb([128, 16, 64], F32, "t1"); R_t1 = Res()
                t2 = ph.sb([128, 16, 64], F32, "t2"); R_t2 = Res()

            def project(c0, ncols):
                for n0 in range(0, ncols, 512):
                    nn = min(512, ncols - n0)
                    for kc in range(8):
                        kb.op("pe", lambda pe, n0=n0, nn=nn, kc=kc: pe.matmul(
                            p_pr[:, n0:n0 + nn], lhsT=xT[:, kc, :], rhs=wq[:, kc, c0 + n0:c0 + n0 + nn],
                            start=(kc == 0), stop=(kc == 7)),
                            reads=[R_xT, R_wq], writes=[R_ppr], disjoint=True)

            def transpose_out(nheads, dst_d, R_dst, t0):
                for h in range(nheads):
                    kb.op("pe", lambda pe, h=h: pe.transpose(out=p_tr[:, h, :], in_=qb[:, h, :], identity=ident_b[:]),
                          reads=[R_qb, R_const], writes=[R_ptr], disjoint=True)
                kb.op("act", lambda a: a.activation(func=AF.Copy, out=qT[:, 0:nheads, :], in_=p_tr[:, 0:nheads, :]), reads=[R_ptr], writes=[R_qT])
                kb.dma(ds_qT, dst_d[:, 0:nheads, t0:t0 + 128], qT[:, 0:nheads, :],
                       reads=[R_qT], writes=[R_dst])

            def norm_rope(nheads, g0, s):
                pv = p_pr[:, 0:nheads * 64].rearrange("p (h d) -> p h d", d=64)
                kb.op("act", lambda a: a.activation(out=sq[:, 0:nheads, :], in_=pv, func=AF.Square), reads=[R_ppr], writes=[R_sq])
                kb.op("dve", lambda v: v.tensor_reduce(out=ssum[:, 0:nheads], in_=sq[:, 0:nheads, :], axis=AX.X, op=ALU.add),
                      reads=[R_sq], writes=[R_ss])
                kb.op("dve", lambda v: v.tensor_scalar(out=ssum[:, 0:nheads], in0=ssum[:, 0:nheads], scalar1=1.0 / 64, scalar2=RMS_EPS,
                                                       op0=ALU.mult, op1=ALU.add), reads=[R_ss], writes=[R_ss])
                kb.op("act", lambda a: a.activation(out=ssum[:, 0:nheads], in_=ssum[:, 0:nheads], func=AF.Sqrt), reads=[R_ss], writes=[R_ss])
                kb.op("dve", lambda v: v.reciprocal(out=ssum[:, 0:nheads], in_=ssum[:, 0:nheads]), reads=[R_ss], writes=[R_ss])
                kb.op("dve", lambda v: v.tensor_tensor(out=qn[:, 0:nheads, :], in0=pv,
                                                       in1=ssum[:, 0:nheads].unsqueeze(2).broadcast_to([128, nheads, 64]), op=ALU.mult),
                      reads=[R_ppr, R_ss], writes=[R_qn])
                kb.op("pool", lambda v: v.tensor_tensor(out=qn[:, 0:nheads, :], in0=qn[:, 0:nheads, :], in1=gain[:, g0:g0 + nheads, :], op=ALU.mult),
                      reads=[R_qn, R_gain], writes=[R_qn])
                kb.op("pool", lambda v: v.tensor_tensor(out=t1[:, 0:nheads, :], in0=qn[:, 0:nheads, :],
                                                        in1=ct[s][:].unsqueeze(1).broadcast_to([128, nheads, 64]), op=ALU.mult),
                      reads=[R_qn, R_cs[s]], writes=[R_t1])
                q5 = qn[:, 0:nheads, :].rearrange("p h (a f j) -> p h a f j", a=2, f=2)
                t5 = t2[:, 0:nheads, :].rearrange("p h (a f j) -> p h a f j", a=2, f=2)
                s5 = sn[s][:].rearrange("p (a f j) -> p a f j", a=2, f=2)
                for hf in range(2):
                    kb.op("dve", lambda v, hf=hf: v.tensor_tensor(
                        out=t5[:, :, :, hf, :], in0=q5[:, :, :, 1 - hf, :],
                        in1=s5[:, :, hf, :].unsqueeze(1).broadcast_to([128, nheads, 2, 16]), op=ALU.mult),
                        reads=[R_qn, R_cs[s]], writes=[R_t2], disjoint=True)
                kb.op("dve", lambda v: v.tensor_tensor(out=qb[:, 0:nheads, :], in0=t1[:, 0:nheads, :], in1=t2[:, 0:nheads, :], op=ALU.add),
                      reads=[R_t1, R_t2], writes=[R_qb])

            for t in range(sub1, min(NT, sub1 + 32) if '1' in PH else 0):
                s = t % 2
                t0 = t * 128
                kb.dma(ds_xt[s], xt[s][:], src_x[t0:t0 + 128, :], reads=[R_src], writes=[R_xt[s]], disjoint=False)
                if is_gqa:
                    kb.dma(ds_cs[s], ct[s][:], rope_c[t0:t0 + 128, :], writes=[R_cs[s]], disjoint=False)
                    kb.dma(ds_cs[s], sn[s][:], rope_s[t0:t0 + 128, :], writes=[R_cs[s]], disjoint=True)
                kb.op("pool", lambda v: v.tensor_copy(out=xb[:], in_=xt[s][:]), reads=[R_xt[s]], writes=[R_xb])
                for kc in range(8):
                    kb.op("pe", lambda pe, kc=kc: pe.transpose(out=p_xT[:, kc, :], in_=xb[:, kc * 128:(kc + 1) * 128], identity=ident_b[:]),
                          reads=[R_xb, R_const], writes=[R_pxT], disjoint=True)
                kb.op("act", lambda a: a.activation(func=AF.Copy, out=xT[:], in_=p_xT[:]), reads=[R_pxT], writes=[R_xT])
                if is_gqa:
                    project(0, 1024)
                    norm_rope(16, 0, s)
                    transpose_out(16, QT_d, R_QT, t0)
                    project(1024, 512)
                    norm_rope(4, 16, s)
                    kb.op("act", lambda a: a.activation(func=AF.Copy, out=vb[:, 0:256], in_=p_pr[:, 256:512]), reads=[R_ppr], writes=[R_vb])
                    transpose_out(4, KT_d, R_KT, t0)
                    kb.dma(ds_vb, V_d[t0:t0 + 128, 0:256], vb[:, 0:256], reads=[R_vb], writes=[R_V])
                else:
                    project(0, 1024)
                    kb.op("act", lambda a: a.activation(func=AF.Copy, out=qb[:].rearrange("p h d -> p (h d)"), in_=p_pr[:, :]), reads=[R_ppr], writes=[R_qb])
                    transpose_out(16, QT_d, R_QT, t0)
                    project(1024, 1024)
                    kb.op("act", lambda a: a.activation(func=AF.Copy, out=qb[:].rearrange("p h d -> p (h d)"), in_=p_pr[:, :]), reads=[R_ppr], writes=[R_qb])
                    transpose_out(16, KT_d, R_KT, t0)
                    project(2048, 1024)
                    kb.op("act", lambda a: a.activation(func=AF.Copy, out=vb[:], in_=p_pr[:, :]), reads=[R_ppr], writes=[R_vb])
                    kb.dma(ds_vb, V_d[t0:t0 + 128, :], vb[:], reads=[R_vb], writes=[R_V])
            ph.close()

        hstep = 1 if is_gqa else 4
        for hg in range(0, NH, hstep):
            ph = Phase()
            if is_gqa:
                KT = ph.sb([64, T], BF16, "KT"); R_KTs = Res(); ds_KT = kb.dsem("KT")
                VA = ph.sb([128, NC, 128], BF16, "VA"); R_VA = Res(); ds_VA = kb.dsem("VA")
                tvt = ph.sb([128, NC], F32, "tvt"); R_tv = Res(); ds_tv = kb.dsem("tv")
                QT = [ph.sb([64, T], BF16, "QT") for _ in range(2)]; R_QTs = [Res(), Res()]; ds_QT = [kb.dsem("QT"), kb.dsem("QT")]
                p_s = [ph.ps([128, 2, 512], F32, "ps") for _ in range(2)]; R_ps = [Res(), Res()]
                p_o = [ph.ps([128, 512], F32, "po") for _ in range(2)]; R_po = [Res(), Res()]
                PT = [ph.sb([128, 2, 512], BF16, "PT") for _ in range(3)]; R_PT = [Res(), Res(), Res()]
                rec = ph.sb([128, 512], F32, "rec"); R_rec = Res()
                on = [ph.sb([64, 512], BF16, "on") for _ in range(2)]; R_on = [Res(), Res()]; ds_on = [kb.dsem("on"), kb.dsem("on")]
                kb.dma(ds_tv, tvt[:], tv_in[:, :], writes=[R_tv])
                for c0 in range(0, NC, 32):
                    c1 = min(NC, c0 + 32)
                    kb.op("pool", lambda v, c0=c0, c1=c1: v.memset(VA[:, c0:c1, :], 1.0), writes=[R_VA], disjoint=(c0 > 0))
                gi = 0
                qi = 0
                for h in range(hg, hg + hstep if '2' in PH else hg):
                    g = h // 4
                    if h % 4 == 0 or h == hg:
                        for c0 in range(0, T, 4096):
                            c1 = min(T, c0 + 4096)
                            kb.dma(ds_KT, KT[:, c0:c1], KT_d[:, g, c0:c1], reads=[R_KT], writes=[R_KTs], disjoint=(c0 > 0))
                        for c0 in range(0, NC, 16):
                            c1 = min(NC, c0 + 16)
                            kb.dma(ds_VA, VA[:, c0:c1, 0:64], V_d[c0 * 128:c1 * 128, g * 64:(g + 1) * 64].rearrange("(c p) d -> p c d", p=128),
                                   reads=[R_V], writes=[R_VA], disjoint=(c0 > 0))
                        for c0 in range(0, NC, 32):
                            c1 = min(NC, c0 + 32)
                            kb.op("dve", lambda v, c0=c0, c1=c1: v.tensor_tensor(out=VA[:, c0:c1, :], in0=VA[:, c0:c1, :],
                                                                                 in1=tvt[:, c0:c1].unsqueeze(2).broadcast_to([128, c1 - c0, 128]), op=ALU.mult),
                                  reads=[R_VA, R_tv], writes=[R_VA])
                    hs = h % 2
                    for c0 in range(0, T, 4096):
                        c1 = min(T, c0 + 4096)
                        kb.dma(ds_QT[hs], QT[hs][:, c0:c1], QT_d[:, h, c0:c1], reads=[R_QT], writes=[R_QTs[hs]], disjoint=(c0 > 0))
                    for qt in range(T // 512):
                        po = qi % 2
                        qi += 1
                        for grp in range(NC // 2):
                            sp_ = gi % 2
                            pt_ = gi % 3
                            gi += 1
                            for j in range(2):
                                c = grp * 2 + j
                                kb.op("pe", lambda pe, c=c, j=j: pe.matmul(p_s[sp_][:, j, :], lhsT=KT[:, c * 128:(c + 1) * 128],
                                                                          rhs=QT[hs][:, qt * 512:(qt + 1) * 512], start=True, stop=True),
                                      reads=[R_KTs, R_QTs[hs]], writes=[R_ps[sp_]], disjoint=True)
                            kb.op("act", lambda a: a.activation(out=PT[pt_][:], in_=p_s[sp_][:], func=AF.Exp, scale=0.125),
                                  reads=[R_ps[sp_]], writes=[R_PT[pt_]])
                            for j in range(2):
                                c = grp * 2 + j
                                kb.op("pe", lambda pe, c=c, j=j: pe.matmul(p_o[po][:], lhsT=VA[:, c, :], rhs=PT[pt_][:, j, :],
                                                                          start=(c == 0), stop=(c == NC - 1)),
                                      reads=[R_VA, R_PT[pt_]], writes=[R_po[po]], disjoint=True)
                        kb.op("dve", lambda v: v.reciprocal(out=rec[64:128, :], in_=p_o[po][64:128, :]), reads=[R_po[po]], writes=[R_rec])
                        kb.op("dve", lambda v: v.tensor_tensor(out=on[po][:], in0=p_o[po][0:64, :], in1=rec[64:128, :], op=ALU.mult),
                              reads=[R_po[po], R_rec], writes=[R_on[po]])
                        kb.dma(ds_on[po], OT_d[(h % 2) * 64:(h % 2) * 64 + 64, h // 2, qt * 512:(qt + 1) * 512], on[po][:], reads=[R_on[po]], writes=[R_OT])
            else:
                KTp = ph.sb([64, T + 512], BF16, "KTp"); R_KTs = Res(); ds_KT = kb.dsem("KT")
                VA = ph.sb([128, NC + 4, 128], BF16, "VAp"); R_VA = Res(); ds_VA = kb.dsem("VA")
                QT1 = ph.sb([64, T], BF16, "QT"); QT = [QT1, QT1]; R_q1 = Res(); R_QTs = [R_q1, R_q1]; ds_q1 = kb.dsem("QT"); ds_QT = [ds_q1, ds_q1]
                Mt = ph.sb([128, 15, 64], F32, "Mt"); R_M = Res(); ds_M = kb.dsem("M")
                mc = ph.sb([128, 2, 64], F32, "mc"); R_mc = Res(); ds_mc = kb.dsem("mc")
                Bfull = ph.sb([128, 6, 256], F32, "Bfull"); R_Bf = Res()
                rv = ph.sb([128, 4, 1536], BF16, "rv"); R_rv = Res(); ds_rv = kb.dsem("rv")
                ngt = ph.sb([128, 1536], F32, "ngt"); R_ngt = Res()
                Bt = ph.sb([128, 4, 1536], F32, "Bt"); R_Bt = Res()
                p_s = [ph.ps([128, 6, 256], F32, "ps") for _ in range(2)]; R_ps = [Res(), Res()]
                p_o = [ph.ps([128, 256], F32, "po") for _ in range(2)]; R_po = [Res(), Res()]
                sbt1 = ph.sb([128, 1536], F32, "sbt"); sbt = [sbt1, sbt1]; R_sb1 = Res(); R_sbt = [R_sb1, R_sb1]
                PT = [ph.sb([128, 6, 256], BF16, "PT") for _ in range(2)]; R_PT = [Res(), Res()]
                rec = ph.sb([128, 256], F32, "rec"); R_rec = Res()
                on = [ph.sb([64, 256], BF16, "on") for _ in range(2)]; R_on = [Res(), Res()]; ds_on = [kb.dsem("on"), kb.dsem("on")]
                kb.dma(ds_mc, mc[:], na_mc.rearrange("a p q -> p a q"), writes=[R_mc])
                kb.dma(ds_rv, rv[:], na_rv.rearrange("a p n -> p a n"), writes=[R_rv])
                kb.op("pool", lambda v: v.memset(KTp[:, 0:256], 0.0), writes=[R_KTs])
                kb.op("pool", lambda v: v.memset(KTp[:, 256 + T:512 + T], 0.0), writes=[R_KTs], disjoint=True)
                for c0 in range(0, NC + 4, 32):
                    c1 = min(NC + 4, c0 + 32)
                    kb.op("pool", lambda v, c0=c0, c1=c1: v.memset(VA[:, c0:c1, :], 1.0), writes=[R_VA], disjoint=(c0 > 0))
                kb.op("pool", lambda v: v.memset(VA[:, 0:2, 0:64], 0.0), writes=[R_VA])
                kb.op("pool", lambda v: v.memset(VA[:, NC + 2:NC + 4, 0:64], 0.0), writes=[R_VA])
                bi = 0
                for h in range(hg, hg + hstep if '2' in PH else hg):
                    hs = h % 2
                    for c0 in range(0, T, 4096):
                        c1 = min(T, c0 + 4096)
                        kb.dma(ds_KT, KTp[:, 256 + c0:256 + c1], KT_d[:, h, c0:c1], reads=[R_KT], writes=[R_KTs], disjoint=(c0 > 0))
                    for c0 in range(0, NC, 16):
                        c1 = min(NC, c0 + 16)
                        kb.dma(ds_VA, VA[:, 2 + c0:2 + c1, 0:64], V_d[c0 * 128:c1 * 128, h * 64:(h + 1) * 64].rearrange("(c p) d -> p c d", p=128),
                               reads=[R_V], writes=[R_VA], disjoint=(c0 > 0))
                    for c0 in range(0, T, 4096):
                        c1 = min(T, c0 + 4096)
                        kb.dma(ds_QT[hs], QT[hs][:, c0:c1], QT_d[:, h, c0:c1], reads=[R_QT], writes=[R_QTs[hs]], disjoint=(c0 > 0))
                    for a in range(2):
                        kb.dma(ds_M, Mt[a * 64:(a + 1) * 64, :, :], na_m[lj, h].rearrange("r k q -> k r q"), writes=[R_M], disjoint=(a == 1))
                    kb.op("dve", lambda v: v.tensor_tensor(out=Mt[:], in0=Mt[:], in1=mc[:, 0:1, :].broadcast_to([128, 15, 64]), op=ALU.mult),
                          reads=[R_M, R_mc], writes=[R_M])
                    kb.op("dve", lambda v: v.tensor_tensor(out=Mt[:], in0=Mt[:], in1=mc[:, 1:2, :].broadcast_to([128, 15, 64]), op=ALU.add),
                          reads=[R_M, R_mc], writes=[R_M])
                    k = 0
                    for c in range(6):
                        for a in range(2):
                            for i in range(4):
                                dr = 2 * c + a - i + 3
                                eng = "pool" if k % 2 == 0 else "dve"
                                k += 1
                                kb.op(eng, lambda v, c=c, a=a, i=i, dr=dr: v.tensor_copy(
                                    out=Bfull[a * 64:(a + 1) * 64, c, i * 64:(i + 1) * 64], in_=Mt[a * 64:(a + 1) * 64, dr, :]),
                                    reads=[R_M], writes=[R_Bf], disjoint=True)
                    bff = Bfull[:].rearrange("p c q -> p (c q)")
                    for ty in range(4):
                        kb.op("dve", lambda v, ty=ty: v.tensor_tensor(out=Bt[:, ty, :], in0=bff, in1=rv[:, ty, :], op=ALU.mult),
                              reads=[R_Bf, R_rv], writes=[R_Bt], disjoint=True)
                        kb.op("dve", lambda v, ty=ty: v.tensor_scalar(out=ngt[:], in0=rv[:, ty, :], scalar1=30000.0, scalar2=-30000.0, op0=ALU.mult, op1=ALU.add),
                              reads=[R_rv], writes=[R_ngt])
                        kb.op("dve", lambda v, ty=ty: v.tensor_tensor(out=Bt[:, ty, :], in0=Bt[:, ty, :], in1=ngt[:], op=ALU.add),
                              reads=[R_Bt, R_ngt], writes=[R_Bt], disjoint=True)
                    for j in range(NB):
                        s_ = bi % 2
                        bi += 1
                        ty = 1 if j == 0 else (2 if j == NB // 2 - 1 else (3 if j == NB - 1 else 0))
                        for c in range(6):
                            k0 = (2 * j + c) * 128
                            kb.op("pe", lambda pe, c=c, k0=k0: pe.matmul(p_s[s_][:, c, :], lhsT=KTp[:, k0:k0 + 128],
                                                                        rhs=QT[hs][:, j * 256:(j + 1) * 256], start=True, stop=True),
                                  reads=[R_KTs, R_QTs[hs]], writes=[R_ps[s_]], disjoint=True)
                        kb.op("dve", lambda v: v.scalar_tensor_tensor(out=sbt[s_][:], in0=p_s[s_][:].rearrange("p c q -> p (c q)"), scalar=0.125,
                                                                      in1=Bt[:, ty, :], op0=ALU.mult, op1=ALU.add),
                              reads=[R_ps[s_], R_Bt], writes=[R_sbt[s_]])
                        kb.op("act", lambda a_: a_.activation(out=PT[s_][:].rearrange("p c q -> p (c q)"), in_=sbt[s_][:], func=AF.Exp),
                              reads=[R_sbt[s_]], writes=[R_PT[s_]])
                        for c in range(6):
                            kb.op("pe", lambda pe, c=c: pe.matmul(p_o[s_][:], lhsT=VA[:, 2 * j + c, :], rhs=PT[s_][:, c, :],
                                                                  start=(c == 0), stop=(c == 5)),
                                  reads=[R_VA, R_PT[s_]], writes=[R_po[s_]], disjoint=True)
                        kb.op("dve", lambda v: v.reciprocal(out=rec[64:128, :], in_=p_o[s_][64:128, :]), reads=[R_po[s_]], writes=[R_rec])
                        kb.op("dve", lambda v: v.tensor_tensor(out=on[s_][:], in0=p_o[s_][0:64, :], in1=rec[64:128, :], op=ALU.mult),
                              reads=[R_po[s_], R_rec], writes=[R_on[s_]])
                        kb.dma(ds_on[s_], OT_d[(h % 2) * 64:(h % 2) * 64 + 64, h // 2, j * 256:(j + 1) * 256], on[s_][:], reads=[R_on[s_]], writes=[R_OT])
            ph.close()

        for sub3 in range(0, NT, 32):
            ph = Phase()
            wo = ph.sb([128, 8, D], BF16, "wo"); R_wo = Res()
            stage = [ph.sb([128, 1024], F32, "stg") for _ in range(2)]; R_stage = [Res(), Res()]
            ds_stage = [kb.dsem("stg"), kb.dsem("stg")]
            load_w_bf16(ph, (gqa_w_o if is_gqa else na_w_o)[lj], D, wo, R_wo, stage, R_stage, ds_stage)
            wr = ph.sb([128, 8, E], F32, "wr"); R_tab = Res(); ds_tab = kb.dsem("tab")
            kb.dma(ds_tab, wr[:], router_w[li].rearrange("(k p) e -> p k e", p=128), writes=[R_tab])
            brt = ph.sb([128, E], F32, "brt")
            bcast_load(ds_tab, brt[:], router_b[li:li + 1, :], E, R_tab)
            g1 = ph.sb([128, D], F32, "g1"); b1 = ph.sb([128, D], F32, "b1")
            bcast_load(ds_tab, g1[:], ln1_g[li:li + 1, :], D, R_tab)
            bcast_load(ds_tab, b1[:], ln1_b[li:li + 1, :], D, R_tab)
            bdn = ph.sb([E, D], F32, "bdn")
            kb.dma(ds_tab, bdn[:], b_dn[li], writes=[R_tab])
            OTt = [ph.sb([128, 8, 128], BF16, "OTt") for _ in range(2)]; R_OTt = [Res(), Res()]; ds_OTt = [kb.dsem("OTt"), kb.dsem("OTt")]
            xt = [ph.sb([128, D], F32, "xt") for _ in range(2)]; R_xt = [Res(), Res()]; ds_xt = [kb.dsem("xt"), kb.dsem("xt")]
            z = ph.sb([128, D], F32, "z"); R_z = Res()
            x1 = [ph.sb([128, D], F32, "x1") for _ in range(2)]; R_x1t = [Res(), Res()]; ds_x1 = [kb.dsem("x1"), kb.dsem("x1")]
            x1b = ph.sb([128, D], BF16, "x1b"); R_x1b = Res()
            x1T = ph.sb([128, 8, 128], BF16, "x1T"); R_x1Ts = Res(); ds_x1T = kb.dsem("x1T")
            x1Tf = ph.sb([128, 8, 128], F32, "x1Tf"); R_x1Tf = Res()
            lg = ph.sb([128, E], F32, "lg"); R_lg = Res()
            m8 = ph.sb([128, 8], F32, "m8"); msk = ph.sb([128, E], F32, "msk"); ex = ph.sb([128, E], F32, "ex")
            nmx = ph.sb([128, 1], F32, "nmx"); ssm = ph.sb([128, 1], F32, "ssm"); R_r = Res()
            gt = [ph.sb([128, E], F32, "gt") for _ in range(2)]; R_gt = [Res(), Res()]; ds_gt = [kb.dsem("gt"), kb.dsem("gt")]
            gT = ph.sb([E, 128], F32, "gT"); R_gT = Res()
            ai = [ph.sb([128, D], F32, "ai") for _ in range(2)]; R_ai = [Res(), Res()]; ds_ai = [kb.dsem("ai"), kb.dsem("ai")]
            st6 = ph.sb([128, 2, 6], F32, "st6"); mv = ph.sb([128, 2], F32, "mv"); rstd = ph.sb([128, 1], F32, "rstd"); R_lnt = Res()
            p_y = ph.ps([128, D], F32, "py"); R_py = Res()
            p_xb = ph.ps([128, 8, 128], BF16, "pxb"); R_pxb = Res()
            p_xf = ph.ps([128, 8, 128], F32, "pxf"); R_pxf = Res()
            p_l = ph.ps([128, 512], F32, "pl"); R_pl = Res()
            p_a = ph.ps([128, D], F32, "pa"); R_pa = Res()
            for t in range(sub3, min(NT, sub3 + 32) if '3' in PH else 0):
                s = t % 2
                t0 = t * 128
                kb.dma(ds_OTt[s], OTt[s][:], OT_d[:, :, t0:t0 + 128], reads=[R_OT], writes=[R_OTt[s]], disjoint=False)
                kb.dma(ds_xt[s], xt[s][:], src_x[t0:t0 + 128, :], reads=[R_src], writes=[R_xt[s]], disjoint=False)
                for nh in range(2):
                    for kc in range(8):
                        kb.op("pe", lambda pe, nh=nh, kc=kc: pe.matmul(p_y[:, nh * 512:(nh + 1) * 512], lhsT=OTt[s][:, kc, :],
                                                                      rhs=wo[:, kc, nh * 512:(nh + 1) * 512], start=(kc == 0), stop=(kc == 7)),
                              reads=[R_OTt[s], R_wo], writes=[R_py], disjoint=True)
                kb.op("dve", lambda v: v.scalar_tensor_tensor(out=z[:], in0=xt[s][:], scalar=float(alpha), in1=p_y[:], op0=ALU.mult, op1=ALU.add),
                      reads=[R_xt[s], R_py], writes=[R_z])
                layer_norm(z, R_z, x1[s], R_x1t[s], g1, b1, R_tab, (st6, mv, rstd, R_lnt))
                kb.dma(ds_x1[s], x1_d[t0:t0 + 128, :], x1[s][:], reads=[R_x1t[s]], writes=[R_x1])
                kb.op("act", lambda a: a.activation(func=AF.Copy, out=x1b[:], in_=x1[s][:]), reads=[R_x1t[s]], writes=[R_x1b])
                for kc in range(8):
                    kb.op("pe", lambda pe, kc=kc: pe.transpose(out=p_xb[:, kc, :], in_=x1b[:, kc * 128:(kc + 1) * 128], identity=ident_b[:]),
                          reads=[R_x1b, R_const], writes=[R_pxb], disjoint=True)
                kb.op("act", lambda a: a.activation(func=AF.Copy, out=x1T[:], in_=p_xb[:]), reads=[R_pxb], writes=[R_x1Ts])
                kb.dma(ds_x1T, x1T_d[:, :, t0:t0 + 128], x1T[:], reads=[R_x1Ts], writes=[R_x1T])
                for kc in range(8):
                    kb.op("pe", lambda pe, kc=kc: pe.transpose(out=p_xf[:, kc, :], in_=x1[s][:, kc * 128:(kc + 1) * 128], identity=ident_f[:]),
                          reads=[R_x1t[s], R_const], writes=[R_pxf], disjoint=True)
                kb.op("dve", lambda v: v.tensor_copy(out=x1Tf[:], in_=p_xf[:]), reads=[R_pxf], writes=[R_x1Tf])
                for kc in range(8):
                    kb.op("pe", lambda pe, kc=kc: pe.matmul(p_l[:, 0:E], lhsT=x1Tf[:, kc, :], rhs=wr[:, kc, :], start=(kc == 0), stop=(kc == 7)),
                          reads=[R_x1Tf, R_tab], writes=[R_pl], disjoint=True)
                kb.op("dve", lambda v: v.tensor_tensor(out=lg[:], in0=p_l[:, 0:E], in1=brt[:], op=ALU.add), reads=[R_pl, R_tab], writes=[R_lg])
                kb.op("dve", lambda v: v.max(out=m8[:], in_=lg[:]), reads=[R_lg], writes=[R_r])
                kb.op("dve", lambda v: v.tensor_scalar(out=msk[:], in0=lg[:], scalar1=m8[:, 3:4], scalar2=None, op0=ALU.is_ge), reads=[R_lg, R_r], writes=[R_r], disjoint=True)
                kb.op("dve", lambda v: v.tensor_scalar(out=nmx[:], in0=m8[:, 0:1], scalar1=-1.0, scalar2=None, op0=ALU.mult), reads=[R_r], writes=[R_r], disjoint=True)
                kb.op("act", lambda a: a.activation(out=ex[:], in_=lg[:], func=AF.Exp, bias=nmx[:, 0:1], scale=1.0), reads=[R_lg, R_r], writes=[R_r], disjoint=True)
                kb.op("dve", lambda v: v.tensor_tensor(out=ex[:], in0=ex[:], in1=msk[:], op=ALU.mult), reads=[R_r], writes=[R_r])
                kb.op("dve", lambda v: v.tensor_reduce(out=ssm[:], in_=ex[:], axis=AX.X, op=ALU.add), reads=[R_r], writes=[R_r])
                kb.op("dve", lambda v: v.reciprocal(out=ssm[:], in_=ssm[:]), reads=[R_r], writes=[R_r])
                kb.op("dve", lambda v: v.tensor_scalar(out=gt[s][:], in0=ex[:], scalar1=ssm[:, 0:1], scalar2=None, op0=ALU.mult), reads=[R_r], writes=[R_gt[s]])
                kb.dma(ds_gt[s], gates_d[t0:t0 + 128, :], gt[s][:], reads=[R_gt[s]], writes=[R_gates])
                kb.op("pe", lambda pe: pe.transpose(out=p_l[0:E, 128:256], in_=gt[s][:], identity=ident_f[:]),
                      reads=[R_gt[s], R_const, R_lg], writes=[R_pl])
                kb.op("dve", lambda v: v.tensor_copy(out=gT[:], in_=p_l[0:E, 128:256]), reads=[R_pl], writes=[R_gT])
                for nh in range(2):
                    kb.op("pe", lambda pe, nh=nh: pe.matmul(p_a[:, nh * 512:(nh + 1) * 512], lhsT=gT[:], rhs=bdn[:, nh * 512:(nh + 1) * 512], start=True, stop=True),
                          reads=[R_gT, R_tab], writes=[R_pa], disjoint=True)
                kb.op("act", lambda a: a.activation(func=AF.Copy, out=ai[s][:], in_=p_a[:]), reads=[R_pa], writes=[R_ai[s]])
                kb.dma(ds_ai[s], acci_d[t0:t0 + 128, :], ai[s][:], reads=[R_ai[s]], writes=[R_acci])
            ph.close()

        ph = Phase()
        sg = [ph.sb([128, 2 * D], F32, "sg") for _ in range(3)]; R_sg = [Res() for _ in range(3)]; ds_sg = [kb.dsem("sg") for _ in range(3)]
        cb = [ph.sb([128, 2, D], BF16, "cb") for _ in range(3)]; R_cb = [Res() for _ in range(3)]; ds_cb = [kb.dsem("cb") for _ in range(3)]
        engs = ["dve", "pool", "act"]
        i3 = 0
        for e in range(E if '4' in PH else 0):
            for kc in range(8):
                s = i3 % 3
                i3 += 1
                kb.dma(ds_sg[s], sg[s][:], w_gu[li, e, kc * 128:(kc + 1) * 128, :], writes=[R_sg[s]], disjoint=False)
                src = sg[s][:].rearrange("p (f two) -> p two f", two=2)
                if engs[s] == "act":
                    kb.op("act", lambda a, s=s, src=src: a.activation(func=AF.Copy, out=cb[s][:], in_=src), reads=[R_sg[s]], writes=[R_cb[s]])
                else:
                    kb.op(engs[s], lambda v, s=s, src=src: v.tensor_copy(out=cb[s][:], in_=src), reads=[R_sg[s]], writes=[R_cb[s]])
                kb.dma(ds_cb[s], wgu_b[e, :, kc * 2 * D:(kc + 1) * 2 * D], cb[s][:].rearrange("p a f -> p (a f)"),
                       reads=[R_cb[s]], writes=[R_wgu])
            for kc in range(0, 8, 2):
                s = i3 % 3
                i3 += 1
                kb.dma(ds_sg[s], sg[s][:].rearrange("p (a f) -> p a f", a=2),
                       w_dn[li, e, kc * 128:(kc + 2) * 128, :].rearrange("(a p) f -> p a f", p=128), writes=[R_sg[s]], disjoint=False)
                src = sg[s][:].rearrange("p (a f) -> p a f", a=2)
                if engs[s] == "act":
                    kb.op("act", lambda a, s=s, src=src: a.activation(func=AF.Copy, out=cb[s][:], in_=src), reads=[R_sg[s]], writes=[R_cb[s]])
                else:
                    kb.op(engs[s], lambda v, s=s, src=src: v.tensor_copy(out=cb[s][:], in_=src), reads=[R_sg[s]], writes=[R_cb[s]])
                kb.dma(ds_cb[s], wd_b[e, :, kc * D:(kc + 2) * D], cb[s][:].rearrange("p a f -> p (a f)"),
                       reads=[R_cb[s]], writes=[R_wd])
        ph.close()

        for sub5 in range(0, T // BLK, 8):
            ph = Phase()
            Wg = [ph.sb([128, 8, 2, D], BF16, "Wg") for _ in range(2)]; R_Wg = [Res(), Res()]; ds_Wg = [kb.dsem("Wg"), kb.dsem("Wg")]
            Wd = [ph.sb([128, 8, D], BF16, "Wd") for _ in range(2)]; R_Wd = [Res(), Res()]; ds_Wd = [kb.dsem("Wd"), kb.dsem("Wd")]
            bgu = ph.sb([128, E, 8, 2], F32, "bgu"); R_tab = Res(); ds_tab = kb.dsem("tab")
            for e in range(E):
                kb.dma(ds_tab, bgu[:, e, :, :], b_gu[li, e].rearrange("(c p two) -> p c two", p=128, two=2), writes=[R_tab])
            g2 = ph.sb([128, D], F32, "g2"); b2 = ph.sb([128, D], F32, "b2")
            bcast_load(ds_tab, g2[:], ln2_g[li:li + 1, :], D, R_tab)
            bcast_load(ds_tab, b2[:], ln2_b[li:li + 1, :], D, R_tab)
            NTB = BLK // 128
            xTb = ph.sb([128, 8, BLK], BF16, "xTb"); R_xTb = Res(); ds_xTb = kb.dsem("xTb")
            gts = ph.sb([128, NTB, E], F32, "gts"); R_gts = Res(); ds_gts = kb.dsem("gts")
            acc = ph.sb([128, NTB, D], F32, "acc"); R_acc = Res(); ds_acc = kb.dsem("acc")
            AT = ph.sb([128, 8, BLK], BF16, "AT"); R_AT = Res()
            gp = [ph.sb([128, BLK], F32, "gp") for _ in range(2)]; R_gp = [Res(), Res()]
            sgm = [ph.sb([128, BLK], F32, "sgm") for _ in range(2)]; R_sgm = [Res(), Res()]
            up = [ph.sb([128, BLK], F32, "up") for _ in range(2)]; R_up = [Res(), Res()]
            x1t = ph.sb([128, D], F32, "x1t"); R_x1tt = Res(); ds_x1t = kb.dsem("x1t")
            xo = [ph.sb([128, D], F32, "xo") for _ in range(2)]; R_xo = [Res(), Res()]; ds_xo = [kb.dsem("xo"), kb.dsem("xo")]
            st6 = ph.sb([128, 2, 6], F32, "st6"); mv = ph.sb([128, 2], F32, "mv"); rstd = ph.sb([128, 1], F32, "rstd"); R_lnt = Res()
            p_g = [ph.ps([128, BLK], F32, "pg") for _ in range(2)]; R_pg = [Res(), Res()]
            p_u = [ph.ps([128, BLK], F32, "pu") for _ in range(2)]; R_pu = [Res(), Res()]
            p_y = [ph.ps([128, 512], F32, "py") for _ in range(2)]; R_pyy = [Res(), Res()]
            dst_x = y_out if li == DEPTH - 1 else xres
            R_dst = R_y if li == DEPTH - 1 else R_xres
            wi = 0; fi = 0; yi = 0; oi = 0
            for tb in range(sub5, min(T // BLK, sub5 + 8) if '5' in PH else sub5):
                b0 = tb * BLK
                kb.dma(ds_xTb, xTb[:], x1T_d[:, :, b0:b0 + BLK], reads=[R_x1T], writes=[R_xTb], disjoint=False)
                kb.dma(ds_gts, gts[:], gates_d[b0:b0 + BLK, :].rearrange("(n p) e -> p n e", p=128), reads=[R_gates], writes=[R_gts], disjoint=False)
                kb.dma(ds_acc, acc[:], acci_d[b0:b0 + BLK, :].rearrange("(n p) d -> p n d", p=128), reads=[R_acci], writes=[R_acc], disjoint=False)
                for e in range(E):
                    w = wi % 2
                    wi += 1
                    kb.dma(ds_Wg[w], Wg[w][:].rearrange("p k a f -> p (k a f)"), wgu_b[e], reads=[R_wgu], writes=[R_Wg[w]], disjoint=False)
                    kb.dma(ds_Wd[w], Wd[w][:].rearrange("p k f -> p (k f)"), wd_b[e], reads=[R_wd], writes=[R_Wd[w]], disjoint=False)
                    for fc in range(8):
                        f = fi % 2
                        fi += 1
                        for kc in range(8):
                            kb.op("pe", lambda pe, kc=kc, fc=fc: pe.matmul(p_g[f][:], lhsT=Wg[w][:, kc, 0, fc * 128:(fc + 1) * 128], rhs=xTb[:, kc, :],
                                                                          start=(kc == 0), stop=(kc == 7)),
                                  reads=[R_Wg[w], R_xTb], writes=[R_pg[f]], disjoint=True)
                        for kc in range(8):
                            kb.op("pe", lambda pe, kc=kc, fc=fc: pe.matmul(p_u[f][:], lhsT=Wg[w][:, kc, 1, fc * 128:(fc + 1) * 128], rhs=xTb[:, kc, :],
                                                                          start=(kc == 0), stop=(kc == 7)),
                                  reads=[R_Wg[w], R_xTb], writes=[R_pu[f]], disjoint=True)
                        kb.op("dve", lambda v, fc=fc: v.tensor_scalar(out=gp[f][:], in0=p_g[f][:], scalar1=bgu[:, e, fc, 0:1], scalar2=7.0, op0=ALU.add, op1=ALU.min),
                              reads=[R_pg[f], R_tab], writes=[R_gp[f]])
                        kb.op("act", lambda a: a.activation(out=sgm[f][:], in_=gp[f][:], func=AF.Sigmoid, scale=1.702), reads=[R_gp[f]], writes=[R_sgm[f]])
                        kb.op("dve", lambda v, fc=fc: v.tensor_scalar(out=up[f][:], in0=p_u[f][:], scalar1=bgu[:, e, fc, 1:2], scalar2=7.0, op0=ALU.add, op1=ALU.min),
                              reads=[R_pu[f], R_tab], writes=[R_up[f]])
                        kb.op("dve", lambda v: v.tensor_scalar(out=up[f][:], in0=up[f][:], scalar1=-7.0, scalar2=1.0, op0=ALU.max, op1=ALU.add),
                              reads=[R_up[f]], writes=[R_up[f]])
                        kb.op("pool", lambda v: v.tensor_tensor(out=gp[f][:], in0=gp[f][:], in1=sgm[f][:], op=ALU.mult), reads=[R_gp[f], R_sgm[f]], writes=[R_gp[f]])
                        kb.op("pool", lambda v, fc=fc: v.tensor_tensor(out=AT[:, fc, :], in0=gp[f][:], in1=up[f][:], op=ALU.mult),
                              reads=[R_gp[f], R_up[f]], writes=[R_AT], disjoint=True)
                    for n in range(NTB):
                        for dh in range(2):
                            y_ = yi % 2
                            yi += 1
                            for fc in range(8):
                                kb.op("pe", lambda pe, fc=fc, n=n, dh=dh: pe.matmul(p_y[y_][:], lhsT=AT[:, fc, n * 128:(n + 1) * 128],
                                                                                  rhs=Wd[w][:, fc, dh * 512:(dh + 1) * 512], start=(fc == 0), stop=(fc == 7)),
                                      reads=[R_AT, R_Wd[w]], writes=[R_pyy[y_]], disjoint=True)
                            kb.op("dve", lambda v, n=n, dh=dh: v.scalar_tensor_tensor(
                                out=acc[:, n, dh * 512:(dh + 1) * 512], in0=p_y[y_][:], scalar=gts[:, n, e:e + 1],
                                in1=acc[:, n, dh * 512:(dh + 1) * 512], op0=ALU.mult, op1=ALU.add),
                                reads=[R_pyy[y_], R_gts, R_acc], writes=[R_acc])
                for n in range(NTB):
                    o = oi % 2
                    oi += 1
                    t0 = b0 + n * 128
                    kb.dma(ds_x1t, x1t[:], x1_d[t0:t0 + 128, :], reads=[R_x1], writes=[R_x1tt], disjoint=False)
                    kb.op("dve", lambda v, n=n: v.scalar_tensor_tensor(out=acc[:, n, :], in0=x1t[:], scalar=float(alpha), in1=acc[:, n, :], op0=ALU.mult, op1=ALU.add),
                          reads=[R_x1tt, R_acc], writes=[R_acc])
                    R_accn = R_acc
                    layer_norm(acc[:, n, :], R_accn, xo[o], R_xo[o], g2, b2, R_tab, (st6, mv, rstd, R_lnt))
                    kb.dma(ds_xo[o], dst_x[t0:t0 + 128, :], xo[o][:], reads=[R_xo[o]], writes=[R_dst])
            ph.close()

    kb.barrier()
    kb.final_wait()
    pc.st.close()
    return nc


def _rope_tables(T):
    t = np.arange(T)
    row = (t // GW).astype(np.float32)
    col = (t % GW).astype(np.float32)
    freqs = (np.float32(10000.0) ** (-np.arange(0, 32, 2, dtype=np.float32) / np.float32(32))).astype(np.float32)
    ar = (row[:, None] * freqs).astype(np.float32)
    ac = (col[:, None] * freqs).astype(np.float32)
    cr, sr, cc, sc = np.cos(ar), np.sin(ar), np.cos(ac), np.sin(ac)
    C = np.concatenate([cr, cr, cc, cc], axis=1).astype(np.float32)
    S = np.concatenate([-sr, sr, -sc, sc], axis=1).astype(np.float32)
    return C, S


def _na_tables(n_rows_valid, T):
    qc = np.arange(64)
    cs = np.clip(qc - 8, 0, 48)
    kc = np.arange(64)
    m = ((kc[:, None] >= cs[None, :]) & (kc[:, None] < cs[None, :] + 16)).astype(np.float32)
    mc = np.stack([np.concatenate([m, m], 0), np.concatenate([(m - 1) * 30000.0] * 2, 0)], 0).astype(np.float32)
    def rv(lo_fn):
        v = np.zeros((128, 6, 256), np.float32)
        for c in range(6):
            for a in range(2):
                kr = 2 * c + a
                for i in range(4):
                    lo, hi = lo_fn(i)
                    if lo <= kr < hi:
                        v[a * 64:(a + 1) * 64, c, i * 64:(i + 1) * 64] = 1.0
        return v
    interior = rv(lambda i: (i, i + 8))
    first = rv(lambda i: (4, 12))
    last = rv(lambda i: (0, 8))
    last_blk = n_rows_valid // 4 - 1
    NBt = T // 256
    tabs = [interior, first, last if last_blk == NBt // 2 - 1 else interior, last if last_blk == NBt - 1 else interior]
    out = np.zeros((4, 128, 1536), np.float32)
    for k, tb in enumerate(tabs):
        out[k] = tb.reshape(128, 1536)
    return mc, out.astype(ml_dtypes.bfloat16)


def _na_m(rpb):
    kc = np.arange(64)[:, None]
    qc = np.arange(64)[None, :]
    idx = np.clip(kc - qc + 15, 0, 30)
    return np.ascontiguousarray(rpb[:, :, :, idx])


_CACHE = {}


def run_cores(seqs, inputs, T, E, DEPTH, alpha, li0=0):
    key = (T, E, DEPTH, float(alpha), li0 % 2)
    if key not in _CACHE:
        _CACHE[key] = build_program(T, E, DEPTH, alpha, li0)
    nc = _CACHE[key]
    C, S = _rope_tables(T)
    na_m = _na_m(np.asarray(inputs["na_rpb"], np.float32))
    shared = {
        "gqa_w_qkv": inputs["gqa_w_qkv"], "gqa_q_norm": inputs["gqa_q_norm"], "gqa_k_norm": inputs["gqa_k_norm"],
        "gqa_w_o": inputs["gqa_w_o"], "na_w_qkv": inputs["na_w_qkv"], "na_m": na_m, "na_w_o": inputs["na_w_o"],
        "ln1_g": inputs["ln1_g"], "ln1_b": inputs["ln1_b"], "ln2_g": inputs["ln2_g"], "ln2_b": inputs["ln2_b"],
        "router_w": inputs["router_w"], "router_b": inputs["router_b"],
        "exp_w_gu": inputs["exp_w_gu"], "exp_b_gu": inputs["exp_b_gu"],
        "exp_w_down": inputs["exp_w_down"], "exp_b_down": inputs["exp_b_down"],
        "rope_c": C, "rope_s": S, "ident_f": np.eye(128, dtype=np.float32),
    }
    shared = {k: np.ascontiguousarray(np.asarray(v, np.float32)) for k, v in shared.items()}
    in_maps = []
    for xs in seqs:
        Sx = xs.shape[0]
        xp = np.zeros((T, D), np.float32)
        xp[:Sx] = xs
        tv = (np.arange(T) < Sx).astype(np.float32).reshape(T // 128, 128).T.copy()
        mc, rvt = _na_tables(Sx // GW, T)
        m = dict(shared)
        m.update({"x": xp, "tv": np.ascontiguousarray(tv), "na_rv": rvt, "na_mc": mc})
        in_maps.append(m)
    res = run_bass_kernel_spmd(nc, in_maps, core_ids=list(range(len(seqs))))
    return [np.asarray(res.results[i]["y"])[: seqs[i].shape[0]] for i in range(len(seqs))]


LAYERS_PER_LAUNCH = 2


def _slice_layers(inputs, li0, n):
    out = {}
    gq = [l // 2 for l in range(li0, li0 + n) if l % 2 == 0]
    na = [l // 2 for l in range(li0, li0 + n) if l % 2 == 1]
    gs = slice(gq[0], gq[-1] + 1) if gq else slice(0, 1)
    ns = slice(na[0], na[-1] + 1) if na else slice(0, 1)
    for k in ("gqa_w_qkv", "gqa_q_norm", "gqa_k_norm", "gqa_w_o"):
        out[k] = np.asarray(inputs[k])[gs]
    for k in ("na_w_qkv", "na_rpb", "na_w_o"):
        out[k] = np.asarray(inputs[k])[ns]
    for k in ("ln1_g", "ln1_b", "ln2_g", "ln2_b", "router_w", "router_b",
              "exp_w_gu", "exp_b_gu", "exp_w_down", "exp_b_down"):
        out[k] = np.asarray(inputs[k])[li0:li0 + n]
    return out


def kernel(**inputs):
    xp = np.asarray(inputs["x_prompt"], np.float32)
    xs = np.asarray(inputs["x_sample"], np.float32)
    DEPTH = 4
    alpha = (2 * DEPTH) ** 0.25
    seqs = [xp[0], xs[0], xs[1]]
    for li0 in range(0, DEPTH, LAYERS_PER_LAUNCH):
        seqs = run_cores(seqs, _slice_layers(inputs, li0, LAYERS_PER_LAUNCH), 16384, 32, LAYERS_PER_LAUNCH, alpha, li0)
    y_prompt = seqs[0][None].astype(np.float32)
    y_sample = np.stack([seqs[1], seqs[2]], 0).astype(np.float32)
    return (y_prompt, y_sample)
```
